# Optimizing a Trainium2 kernel written in Bass

```python
import jax, jax.numpy as jnp
from jax import lax
import numpy as np

D_MODEL = 1024
BATCH = 8
SEQ = 4096
DEPTH = 1

N_MEM = 256
MOBA_HEADS = 8
MOBA_HD = 64
MOBA_W = MOBA_HEADS * MOBA_HD
MOBA_BLOCK = 256
MOBA_TOPK = 3
MOBA_QCHUNK = 32
HGRN_HEADS = 4
HGRN_DK = 128
HGRN_DV = 128
HGRN_FW = HGRN_HEADS * HGRN_DK
HGRN_W = HGRN_HEADS * HGRN_DV
HGRN_CHUNK = 32
XA_HEADS = 4
XA_HD = 128
XA_W = XA_HEADS * XA_HD
N_BRANCH = 3
PROJ_WIDTHS = (MOBA_W, MOBA_W, MOBA_W, MOBA_W,
               HGRN_FW, HGRN_W, HGRN_FW, HGRN_W,
               XA_W, XA_W,
               D_MODEL, D_MODEL, D_MODEL)
PROJ_TOTAL = sum(PROJ_WIDTHS)
EPS = 1e-6

kernel_name = "hybrid_moba_hgrn2_memxattn_gated"


def _rmsnorm(x, w):
    x32 = x.astype(jnp.float32)
    y = x32 * lax.rsqrt(jnp.mean(x32 * x32, axis=-1, keepdims=True) + EPS)
    return (y * w.astype(jnp.float32)).astype(x.dtype)


def _alibi_slopes(n):
    return jnp.exp2(-8.0 * jnp.arange(1, n + 1, dtype=jnp.float32) / n)


def _moba(q, k, v):
    bsz, s, nh, dh = q.shape
    nb = -(-s // MOBA_BLOCK)
    topk = min(MOBA_TOPK, nb)
    scale = dh ** -0.5
    slopes = _alibi_slopes(nh)
    q = q.transpose(0, 2, 1, 3)
    k = k.transpose(0, 2, 1, 3)
    v = v.transpose(0, 2, 1, 3)
    pad = nb * MOBA_BLOCK - s
    kp = jnp.pad(k, ((0, 0), (0, 0), (0, pad), (0, 0)))
    vp = jnp.pad(v, ((0, 0), (0, 0), (0, pad), (0, 0)))
    kb = kp.reshape(bsz, nh, nb, MOBA_BLOCK, dh)
    vb = vp.reshape(bsz, nh, nb, MOBA_BLOCK, dh)
    kbar = jnp.mean(kb.astype(jnp.float32), axis=3)
    pos = jnp.arange(s)
    qblk = pos // MOBA_BLOCK
    gate = jnp.einsum('bhtd,bhnd->bhtn', q.astype(jnp.float32), kbar)
    past = jnp.arange(nb)[None, :] < qblk[:, None]
    gate = jnp.where(past, gate, -jnp.inf)
    gval, gidx = lax.top_k(gate, topk)
    gok = jnp.isfinite(gval)

    nq = s // MOBA_QCHUNK

    def to_chunks(a):
        a = a.reshape(bsz, nh, nq, MOBA_QCHUNK, *a.shape[3:])
        return jnp.moveaxis(a, 2, 0)

    bi = jnp.arange(bsz)[:, None, None, None]
    hi = jnp.arange(nh)[None, :, None, None]
    offs = jnp.arange(MOBA_BLOCK)

    def chunk(args):
        c, qc, ic, okc = args
        t = c * MOBA_QCHUNK + jnp.arange(MOBA_QCHUNK)
        start = (c * MOBA_QCHUNK) // MOBA_BLOCK * MOBA_BLOCK
        ko = lax.dynamic_slice_in_dim(kp, start, MOBA_BLOCK, axis=2)
        vo = lax.dynamic_slice_in_dim(vp, start, MOBA_BLOCK, axis=2)
        so = start + offs
        dist_o = (t[:, None] - so[None, :]).astype(jnp.float32)
        lo = (jnp.einsum('bhqd,bhkd->bhqk', qc, ko).astype(jnp.float32) * scale
              - slopes[:, None, None] * dist_o)
        lo = jnp.where(so[None, :] <= t[:, None], lo, -jnp.inf)
        kg = kb[bi, hi, ic]
        vg = vb[bi, hi, ic]
        sg = ic[..., None] * MOBA_BLOCK + offs
        dist_s = (t[None, None, :, None, None] - sg).astype(jnp.float32)
        ls = (jnp.einsum('bhqd,bhqjkd->bhqjk', qc, kg).astype(jnp.float32) * scale
              - slopes[None, :, None, None, None] * dist_s)
        ls = jnp.where(okc[..., None], ls, -jnp.inf)
        logits = jnp.concatenate(
            [ls.reshape(bsz, nh, MOBA_QCHUNK, topk * MOBA_BLOCK), lo], axis=-1)
        p = jax.nn.softmax(logits, axis=-1)
        ps = p[..., :topk * MOBA_BLOCK].reshape(bsz, nh, MOBA_QCHUNK, topk, MOBA_BLOCK).astype(v.dtype)
        po = p[..., topk * MOBA_BLOCK:].astype(v.dtype)
        return (jnp.einsum('bhqjk,bhqjkd->bhqd', ps, vg)
                + jnp.einsum('bhqk,bhkd->bhqd', po, vo))

    out = lax.map(chunk, (jnp.arange(nq), to_chunks(q), to_chunks(gidx), to_chunks(gok)))
    out = jnp.moveaxis(out, 0, 2).reshape(bsz, nh, s, dh)
    return out.transpose(0, 2, 1, 3).reshape(bsz, s, nh * dh)


def _hgrn2(f_logit, inp, qry, lb):
    bsz, s, nh, dk = f_logit.shape
    dv = inp.shape[-1]
    c = HGRN_CHUNK
    n = s // c
    fl = f_logit.astype(jnp.float32)
    log_f = jnp.log(lb + (1.0 - lb) * jax.nn.sigmoid(fl))
    kk = (1.0 - lb) * jax.nn.sigmoid(-fl)

    def chunks(a):
        return a.reshape(bsz, n, c, nh, a.shape[-1]).transpose(0, 3, 1, 2, 4)

    log_f, kk = chunks(log_f), chunks(kk)
    qq = chunks(qry.astype(jnp.float32))
    vv = chunks(inp.astype(jnp.float32))
    b = jnp.cumsum(log_f, axis=3)
    q_t = qq * jnp.exp(b)
    k_t = kk * jnp.exp(-b)
    a = jnp.einsum('bhnid,bhnjd->bhnij', q_t, k_t)
    tril = jnp.tril(jnp.ones((c, c), dtype=bool))
    a = jnp.where(tril, a, 0.0)
    o_intra = jnp.einsum('bhnij,bhnje->bhnie', a, vv)
    b_last = b[..., -1:, :]
    u = jnp.einsum('bhncd,bhnce->bhnde', kk * jnp.exp(b_last - b), vv)
    decay = jnp.exp(b_last[..., 0, :])

    def step(state, xs):
        dec, uu = xs
        return dec[..., None] * state + uu, state

    s0 = jnp.zeros((bsz, nh, dk, dv), jnp.float32)
    _, s_prev = lax.scan(step, s0, (jnp.moveaxis(decay, 2, 0), jnp.moveaxis(u, 2, 0)))
    o_inter = jnp.einsum('bhnid,nbhde->bhnie', q_t, s_prev)
    o = o_intra + o_inter
    return o.transpose(0, 2, 3, 1, 4).reshape(bsz, s, nh, dv)


def _mem_xattn(q, mem_n, w_kv):
    bsz, s, _ = q.shape
    m = mem_n.shape[1]
    kv = mem_n @ w_kv
    km, vm = jnp.split(kv, 2, axis=-1)
    km = km.reshape(bsz, m, XA_HEADS, XA_HD)
    vm = vm.reshape(bsz, m, XA_HEADS, XA_HD)
    qh = q.reshape(bsz, s, XA_HEADS, XA_HD)
    logits = jnp.einsum('bshd,bmhd->bhsm', qh, km).astype(jnp.float32) * (XA_HD ** -0.5)
    p = jax.nn.softmax(logits, axis=-1).astype(vm.dtype)
    return jnp.einsum('bhsm,bmhd->bshd', p, vm).reshape(bsz, s, XA_W)


def setup_inputs(seed: int = 0) -> dict:
    key = jax.random.key(seed)
    ks = jax.random.split(key, 13)
    f32 = jnp.float32
    nrm = lambda k, shp, sc: jax.random.normal(k, shp, f32) * sc
    return {
        "x": nrm(ks[0], (BATCH, SEQ, D_MODEL), 1.0),
        "mem": nrm(ks[1], (BATCH, N_MEM, D_MODEL), 1.0),
        "pre_norm_w": 1.0 + nrm(ks[2], (DEPTH, D_MODEL), 0.1),
        "w_in": nrm(ks[3], (DEPTH, D_MODEL, PROJ_TOTAL), D_MODEL ** -0.5),
        "hgrn_lb_logits": nrm(ks[4], (DEPTH + 1, HGRN_FW), 0.1),
        "hgrn_norm_w": 1.0 + nrm(ks[5], (DEPTH, HGRN_DV), 0.1),
        "mem_norm_w": 1.0 + nrm(ks[6], (DEPTH, D_MODEL), 0.1),
        "w_mem_kv": nrm(ks[7], (DEPTH, D_MODEL, 2 * XA_W), D_MODEL ** -0.5),
        "w_branch_a": nrm(ks[8], (DEPTH, MOBA_W, D_MODEL), MOBA_W ** -0.5),
        "w_branch_b": nrm(ks[9], (DEPTH, HGRN_W, D_MODEL), HGRN_W ** -0.5),
        "w_branch_c": nrm(ks[10], (DEPTH, XA_W, D_MODEL), XA_W ** -0.5),
        "w_out": nrm(ks[11], (DEPTH, D_MODEL, D_MODEL), D_MODEL ** -0.5),
        "post_norm_w": 1.0 + nrm(ks[12], (DEPTH, D_MODEL), 0.1),
    }


def reference(x, mem, pre_norm_w, w_in, hgrn_lb_logits, hgrn_norm_w, mem_norm_w, w_mem_kv,
              w_branch_a, w_branch_b, w_branch_c, w_out, post_norm_w):
    bsz, s, _ = x.shape
    split_idx = [int(v) for v in np.cumsum(PROJ_WIDTHS)[:-1]]
    lb_all = jnp.cumsum(jax.nn.softmax(hgrn_lb_logits.astype(jnp.float32), axis=0), axis=0)
    for l in range(DEPTH):
        h = _rmsnorm(x, pre_norm_w[l])
        proj = h @ w_in[l]
        (qa, ka, va, za, fb, ib, qb, gb, qc, zc,
         gate_a, gate_b, gate_c) = jnp.split(proj, split_idx, axis=-1)

        hs = lambda t: t.reshape(bsz, s, MOBA_HEADS, MOBA_HD)
        ya = _moba(hs(qa), hs(ka), hs(va)) * jax.nn.silu(za)

        lb = lb_all[l].reshape(HGRN_HEADS, HGRN_DK)
        ob = _hgrn2(fb.reshape(bsz, s, HGRN_HEADS, HGRN_DK),
                    ib.reshape(bsz, s, HGRN_HEADS, HGRN_DV),
                    qb.reshape(bsz, s, HGRN_HEADS, HGRN_DK), lb)
        ob = _rmsnorm(ob, hgrn_norm_w[l]).reshape(bsz, s, HGRN_W).astype(x.dtype)
        yb = ob * jax.nn.silu(gb)

        mem_n = _rmsnorm(mem, mem_norm_w[l])
        yc = _mem_xattn(qc, mem_n, w_mem_kv[l]) * jax.nn.silu(zc)

        merged = (jax.nn.sigmoid(gate_a) * (ya @ w_branch_a[l])
                  + jax.nn.sigmoid(gate_b) * (yb @ w_branch_b[l])
                  + jax.nn.sigmoid(gate_c) * (yc @ w_branch_c[l]))
        y = merged @ w_out[l]
        x = x + _rmsnorm(y, post_norm_w[l])
    return x
```

```python
import numpy as np
import ml_dtypes
import concourse.bass as bass
import concourse.mybir as mybir
from concourse.bass_utils import run_bass_kernel_spmd

F32 = mybir.dt.float32
BF16 = mybir.dt.bfloat16
AF = mybir.ActivationFunctionType
ALU = mybir.AluOpType
AX = mybir.AxisListType

T = 4096
D = 1024
NT = T // 128
NEG = -30000.0
EPS = 1e-6
DEBUG = False
HGRN_FINE = False


def _es(dt):
    return mybir.dt.size(dt)


class Op:
    __slots__ = ("eng", "fn", "deps", "dma", "sig", "cnt", "sem", "target", "waited", "prev_target")

    def __init__(self, eng, fn, deps, dma):
        self.eng = eng
        self.fn = fn
        self.deps = deps
        self.dma = dma
        self.sig = False
        self.cnt = 0
        self.sem = None
        self.target = 0
        self.waited = False


class Sched:
    CE = ("pe", "act", "dve", "pool")

    def __init__(self, nc):
        self.nc = nc
        self.ops = []
        self.kinds = {}
        self.recs = {}
        self.last = {}
        self.barrier_deps = {}
        self.open_dmas = set()

    def reg_tensor(self, handle, kind):
        self.kinds[handle.name] = kind
        return handle

    def region(self, ap):
        name = ap.tensor.name
        kind = self.kinds[name]
        es = _es(ap.dtype)
        pat = list(ap.ap)
        off = int(ap.offset)
        if kind == "dram":
            ext = 1 + sum((c - 1) * abs(s) for s, c in pat)
            return (name, 0, 1, off * es, (off + ext) * es)
        pstep, npart = pat[0]
        p0 = off // pstep
        f0 = off % pstep
        ext = 1 + sum((c - 1) * abs(s) for s, c in pat[1:])
        if kind == "ps":
            return (name, 0, 128, 0, 2048)
        return (name, p0, p0 + npart, f0 * es, (f0 + ext) * es)

    def _scan(self, reg, is_write, eng, idx, deps):
        name, p0, p1, b0, b1 = reg
        lst = self.recs.setdefault(name, [])
        keep = []
        for r in lst:
            rp0, rp1, rb0, rb1, rop, reng, rw = r
            ov = rp0 < p1 and p0 < rp1 and rb0 < b1 and b0 < rb1
            if ov:
                if is_write:
                    deps.append((rop, "WAW" if rw else "WAR"))
                elif rw:
                    deps.append((rop, "RAW"))
            if is_write and ov and p0 <= rp0 and rp1 <= p1 and b0 <= rb0 and rb1 <= b1:
                continue
            if (not is_write) and (not rw) and reng == eng and (rp0, rp1, rb0, rb1) == (p0, p1, b0, b1) \
                    and not self.ops[rop].dma:
                continue
            keep.append(r)
        keep.append((p0, p1, b0, b1, idx, eng, is_write))
        self.recs[name] = keep

    def add(self, eng, fn, reads=(), writes=(), dma=False):
        idx = len(self.ops)
        deps = []
        for a in reads:
            self._scan(self.region(a), False, eng, idx, deps)
        for a in writes:
            self._scan(self.region(a), True, eng, idx, deps)
        if eng in self.barrier_deps:
            for d in self.barrier_deps.pop(eng):
                deps.append((d, "RAW"))
        op = Op(eng, fn, deps, dma)
        self.ops.append(op)
        for d, _k in deps:
            if self.ops[d].dma:
                self.ops[d].waited = True
                self.open_dmas.discard(d)
        if dma:
            self.open_dmas.add(idx)
        else:
            self.last[eng] = idx
        return idx

    def barrier(self):
        deps = set(self.last.values()) | set(self.open_dmas)
        for e in ("pe", "act", "dve", "pool", "sp"):
            self.barrier_deps[e] = set(deps)
        self.recs = {}

    def final_wait(self):
        self.barrier()
        self.add("sp", None)

    def emit(self, block, sems, dma_sems):
        ops = self.ops
        def real(o, d, kind):
            od = ops[d]
            if od is o:
                return False
            if od.dma or o.dma:
                return True
            if od.eng == o.eng and o.eng == "pe":
                return False
            return True

        for o in ops:
            nd = {}
            for d, kind in o.deps:
                if real(o, d, kind):
                    nd[d] = True
            o.deps = sorted(nd.keys())
            for d in o.deps:
                ops[d].sig = True
        cnt = {e: 0 for e in self.CE}
        rr = {q: 0 for q in dma_sems}
        tot = {}
        for o in ops:
            if o.dma:
                pool = dma_sems[o.eng]
                s = pool[rr[o.eng] % len(pool)]
                rr[o.eng] += 1
                o.sem = s
                o.prev_target = tot.get(id(s), 0)
                tot[id(s)] = o.prev_target + 16
                o.target = tot[id(s)]
            elif o.sig and o.fn is not None:
                cnt[o.eng] += 1
                o.cnt = cnt[o.eng]
        per_eng = {e: [] for e in ("pe", "act", "dve", "pool", "sp")}
        for o in ops:
            per_eng[o.eng].append(o)

        def run(e, eh):
            waited_c = {x: 0 for x in self.CE}
            waited_d = {}
            for o in per_eng[e]:
                need_c = {}
                need_d = {}
                for d in o.deps:
                    od = ops[d]
                    if od.dma:
                        k = id(od.sem)
                        if need_d.get(k, (None, 0))[1] < od.target:
                            need_d[k] = (od.sem, od.target)
                    else:
                        if need_c.get(od.eng, 0) < od.cnt:
                            need_c[od.eng] = od.cnt
                if o.dma and o.prev_target > 0:
                    k = id(o.sem)
                    if need_d.get(k, (None, 0))[1] < o.prev_target:
                        need_d[k] = (o.sem, o.prev_target)
                for x, v in need_c.items():
                    if v > waited_c[x]:
                        eh.wait_ge(sems[x], v)
                        waited_c[x] = v
                for k, (s, v) in need_d.items():
                    if v > waited_d.get(k, 0):
                        eh.wait_ge(s, v)
                        waited_d[k] = v
                if o.fn is None:
                    continue
                inst = o.fn(eh)
                if o.dma:
                    inst.then_inc(o.sem, 16)
                elif o.sig:
                    inst.then_inc(sems[e], 1)

        @block.tensor
        def _(eh):
            run("pe", eh)

        @block.scalar
        def _(eh):
            run("act", eh)

        @block.vector
        def _(eh):
            run("dve", eh)

        @block.gpsimd
        def _(eh):
            run("pool", eh)

        @block.sync
        def _(eh):
            run("sp", eh)


class B:
    def __init__(self, nc):
        self.nc = nc
        self.S = Sched(nc)
        self.sb_off = 16512
        self.uid = 0

    def sb(self, name, shape, dt, off=None):
        nbytes = int(np.prod(shape[1:])) * _es(dt)
        if off is None:
            off = (self.sb_off + 63) // 64 * 64
            self.sb_off = off + nbytes
        assert off + nbytes <= 229376, (name, off, nbytes)
        self.uid += 1
        h = self.nc.alloc_sbuf_tensor_at(f"{name}_{self.uid}", list(shape), dt, offset=off)
        return self.S.reg_tensor(h, "sb")

    def ps(self, name):
        h = self.nc.alloc_psum_tensor(name, [128, 512], F32)
        return self.S.reg_tensor(h, "ps")

    def dram(self, name, shape, dt, kind):
        h = self.nc.dram_tensor(name, list(shape), dt, kind=kind)
        self.S.reg_tensor(h, "dram")
        return h.ap()

    def mm(self, out, lhsT, rhs, start=True, stop=True):
        rd = [lhsT, rhs] + ([] if start else [out])
        self.S.add("pe", lambda e: e.matmul(out, lhsT, rhs, start=start, stop=stop), rd, [out])

    def tr(self, out, in_, ident):
        self.S.add("pe", lambda e: e.transpose(out, in_, ident), [in_, ident], [out])

    def act(self, out, in_, func, bias=None, scale=None, accum_out=None):
        rd = [in_]
        kw = {}
        if bias is not None:
            kw["bias"] = bias
            if not isinstance(bias, (int, float)):
                rd.append(bias)
        if scale is not None:
            kw["scale"] = scale
            if not isinstance(scale, (int, float)):
                rd.append(scale)
        wr = [out]
        if accum_out is not None:
            kw["accum_out"] = accum_out
            wr.append(accum_out)
        self.S.add("act", lambda e: e.activation(out, in_, func, **kw), rd, wr)

    def tt(self, out, in0, in1, op, eng="dve"):
        self.S.add(eng, lambda e: e.tensor_tensor(out, in0, in1, op), [in0, in1], [out])

    def ts(self, out, in0, s1, s2, op0, op1=None, eng="dve"):
        rd = [in0]
        for s in (s1, s2):
            if s is not None and not isinstance(s, (int, float)):
                rd.append(s)
        if op1 is None:
            fn = lambda e: e.tensor_scalar(out, in0, s1, None, op0)
        else:
            fn = lambda e: e.tensor_scalar(out, in0, s1, s2, op0, op1)
        self.S.add(eng, fn, rd, [out])

    def stt(self, out, in0, scalar, in1, op0, op1, eng="dve"):
        rd = [in0, in1]
        if not isinstance(scalar, (int, float)):
            rd.append(scalar)
        self.S.add(eng, lambda e: e.scalar_tensor_tensor(out, in0, scalar, in1, op0, op1), rd, [out])

    def copy(self, out, in_, eng="dve"):
        self.S.add(eng, lambda e: e.tensor_copy(out, in_), [in_], [out])

    def memset(self, out, val, eng="dve"):
        self.S.add(eng, lambda e: e.memset(out, val), [], [out])

    def recip(self, out, in_):
        self.S.add("dve", lambda e: e.reciprocal(out, in_), [in_], [out])

    def rsum(self, out, in_):
        self.S.add("dve", lambda e: e.reduce_sum(out, in_, AX.X), [in_], [out])

    def max8(self, out, in_):
        self.S.add("dve", lambda e: e.max(out, in_), [in_], [out])

    def scan(self, out, d0, d1, init, op0, op1):
        self.S.add("dve", lambda e: e.tensor_tensor_scan(out, d0, d1, init, op0, op1), [d0, d1], [out])

    def dma(self, q, out, in_):
        self.S.add(q, lambda e: e.dma_start(out=out, in_=in_), [in_], [out], dma=True)


def bc_last(ap2, n):
    pat = list(ap2.ap)
    return bass.AP(ap2.tensor, ap2.offset, [list(p) for p in pat] + [[0, n]])


def build():
    nc = bass.Bass("TRN2", target_bir_lowering=False)
    b = B(nc)
    S = b.S
    x_d = b.dram("x", [T, D], F32, "ExternalInput")
    mem_d = b.dram("mem", [256, D], F32, "ExternalInput")
    win_d = b.dram("w_in", [D, 8192], F32, "ExternalInput")
    wkv_d = b.dram("w_kv", [D, 1024], F32, "ExternalInput")
    wa_d = b.dram("w_a", [512, D], F32, "ExternalInput")
    wb_d = b.dram("w_b", [512, D], F32, "ExternalInput")
    wc_d = b.dram("w_c", [512, D], F32, "ExternalInput")
    wo_d = b.dram("w_o", [D, D], F32, "ExternalInput")
    prew_d = b.dram("prew", [128, 8], F32, "ExternalInput")
    memw_d = b.dram("memw", [128, 8], F32, "ExternalInput")
    postw_d = b.dram("postw", [128, D], F32, "ExternalInput")
    lbl_d = b.dram("lbl", [128, 8], F32, "ExternalInput")
    hnw_d = b.dram("hnw", [128, 1], F32, "ExternalInput")
    identb_d = b.dram("identb", [128, 128], BF16, "ExternalInput")
    identf_d = b.dram("identf", [128, 128], F32, "ExternalInput")
    trib_d = b.dram("trib", [128, 128], BF16, "ExternalInput")
    trim_d = b.dram("trim", [128, 128], F32, "ExternalInput")
    kac_d = b.dram("kac", [20, T], BF16, "ExternalInput")
    qac_d = b.dram("qac", [8, 4, T], BF16, "ExternalInput")
    pastb_d = b.dram("pastb", [128, 512], F32, "ExternalInput")
    rmask_d = b.dram("rmask", [128, 512], F32, "ExternalInput")
    out_d = b.dram("out", [T, D], F32, "ExternalOutput")
    skind = "ExternalOutput" if DEBUG else "Internal"
    ya_d = b.dram("ya_s", [512, T], BF16, skind)
    yb_d = b.dram("yb_s", [512, T], BF16, skind)

    banks = [b.ps(f"bank{i}") for i in range(8)]

    identb = b.sb("identb", [128, 128], BF16)
    identf = b.sb("identf", [128, 128], F32)
    onesb = b.sb("onesb", [128, 128], BF16)
    prew = b.sb("prew", [128, 8], F32)
    memw = b.sb("memw", [128, 8], F32)
    kmT = b.sb("kmT", [128, 4, 256], BF16)
    vm = b.sb("vm", [128, 2, 512], BF16)
    for dst, src in ((identb, identb_d), (identf, identf_d), (prew, prew_d), (memw, memw_d)):
        b.dma("sp", dst[:], src)
    b.memset(onesb[:], 1.0)
    epsb = b.sb("epsb", [128, 1], F32)
    b.memset(epsb[:], EPS)
    persist_mark = b.sb_off

    xq = [None]

    def norm_tiles(src_d, row0, ntiles, wT, dstT, dst_col0, xts, hb, tp_bank, junk, ss, rstd):
        def stats(i):
            xt = xts[i % len(xts)]
            k = i % 4
            b.act(junk[:], xt[:], AF.Square, accum_out=ss[:, k:k + 1])
            b.act(rstd[:, k:k + 1], ss[:, k:k + 1], AF.Sqrt, bias=epsb[:, 0:1], scale=1.0 / D)
            b.recip(rstd[:, k:k + 1], rstd[:, k:k + 1])

        def mult(i):
            xt = xts[i % len(xts)]
            k = i % 4
            hbi = hb[i % 2]
            b.ts(hbi[:], xt[:], rstd[:, k:k + 1], None, ALU.mult)
            tpv = tp_bank[i % 2][:].bitcast(BF16)
            for kc in range(8):
                b.tr(tpv[:, kc * 128:(kc + 1) * 128], hbi[:, kc * 128:(kc + 1) * 128], identb[:])

        def evac(i):
            tpv = tp_bank[i % 2][:].bitcast(BF16)
            c0 = dst_col0 + i * 128
            b.tt(dstT[:, :, c0:c0 + 128], tpv.rearrange("p (k t) -> p k t", t=128),
                 bc_last(wT[:, :], 128), ALU.mult)

        def load(i):
            b.dma("sp", xts[i % len(xts)][:], src_d[row0 + i * 128: row0 + (i + 1) * 128, :])

        ahead = len(xts) - 2
        for i in range(min(ahead, ntiles)):
            load(i)
        stats(0)
        if ntiles > 1:
            stats(1)
        mult(0)
        for i in range(ntiles):
            if i + ahead < ntiles:
                load(i + ahead)
            if i + 2 < ntiles:
                stats(i + 2)
            if i + 1 < ntiles:
                mult(i + 1)
            evac(i)

    def load_w(dst, src_d, c0, ncols, nk):
        v = src_d.rearrange("(k p) c -> p k c", p=128)
        half = max(1, nk // 2)
        for k0 in range(0, nk, half):
            b.dma("pool", dst[:, k0:k0 + half, :], v[:, k0:k0 + half, c0:c0 + ncols])

    hT = b.sb("hT", [128, 8, T], BF16)
    ph_mark = b.sb_off
    wq = b.sb("wq", [128, 8, 512], BF16)
    wk = b.sb("wk", [128, 8, 512], BF16)
    wv = b.sb("wv", [128, 8, 512], BF16)
    wz = b.sb("wz", [128, 8, 512], BF16)
    a_mark = b.sb_off
    xts = [b.sb(f"xt{i}", [128, D], F32) for i in range(8)]
    hb = [b.sb(f"hb{i}", [128, D], BF16) for i in range(2)]
    junk = b.sb("junk", [128, D], BF16)
    ss = b.sb("ss", [128, 4], F32)
    rstd = b.sb("rstd", [128, 4], F32)
    memT = b.sb("memT", [128, 8, 256], BF16)
    wkv = b.sb("wkv", [128, 8, 1024], BF16)
    load_w(wkv, wkv_d, 0, 1024, 8)
    for i, w in enumerate((wq, wk, wv, wz)):
        load_w(w, win_d, i * 512, 512, 8)
    norm_tiles(mem_d, 0, 2, memw, memT, 0, xts, hb, [banks[6], banks[7]], junk, ss, rstd)
    for h in range(4):
        pb = banks[h % 2]
        for kc in range(8):
            b.mm(pb[:, 0:256], wkv[:, kc, h * 128:(h + 1) * 128], memT[:, kc, :], kc == 0, kc == 7)
        b.copy(kmT[:, h, :], pb[:, 0:256])
    for mt in range(2):
        pb = banks[2 + mt]
        for kc in range(8):
            b.mm(pb[:, :], memT[:, kc, mt * 128:(mt + 1) * 128], wkv[:, kc, 512:1024], kc == 0, kc == 7)
        b.copy(vm[:, mt, :], pb[:, :])
    norm_tiles(x_d, 0, NT, prew, hT, 0, xts, hb, [banks[6], banks[7]], junk, ss, rstd)

    S.barrier()
    b.sb_off = a_mark
    qaug = [b.sb(f"qaug{j}", [84, T], BF16) for j in range(2)]
    kaug = [b.sb(f"kaug{j}", [84, T], BF16) for j in range(2)]
    vP = b.sb("vP", [128, 32, 2, 128], BF16)
    zs = b.sb("zs", [128, T], BF16)
    yaT = b.sb("yaT", [128, T], BF16)
    trib = b.sb("trib", [128, 128], BF16)
    pastb = b.sb("pastb", [128, 512], F32)
    Gs = b.sb("Gs", [128, 512], F32)
    mbias2 = [b.sb(f"mbias{j}", [128, 512], F32) for j in range(2)]
    top8 = b.sb("top8", [128, 32, 8], F32)
    ksum = b.sb("ksum", [64, 32], F32)
    kbarT = [b.sb(f"kbarT{j}", [64, 16], BF16) for j in range(2)]
    PT = [b.sb(f"PT{i}", [128, 512], BF16) for i in range(4)]
    rec = b.sb("rec", [64, 512], F32)
    lnd = b.sb("lnd", [64, 512], F32)
    tmpn = b.sb("tmpn", [128, 512], F32)
    thz = [b.sb(f"thz{i}", [128, 512], F32) for i in range(2)]
    b.dma("sp", trib[:], trib_d)
    b.dma("sp", pastb[:], pastb_d)
    b.memset(vP[:, :, :, 64:128].rearrange("p t j c -> p (t j) c"), 1.0)
    for j in range(2):
        b.dma("sp", kaug[j][64:84, :], kac_d)
    pj = [banks[0], banks[1]]
    sbk = [banks[0], banks[1], banks[2], banks[3]]
    ob = [banks[4], banks[5]]
    gb_ = banks[6]
    tb_ = banks[7]

    for p in range(4):
        cs = slice(p * 128, (p + 1) * 128)
        for j in range(2):
            b.dma("sp", qaug[j][80:84, :], qac_d[2 * p + j])
        for tc in range(8):
            ts_ = slice(tc * 512, (tc + 1) * 512)
            pb = banks[0]
            for kc in range(8):
                b.mm(pb[:, :], wq[:, kc, cs], hT[:, kc, ts_], kc == 0, kc == 7)
            b.act(qaug[0][0:64, ts_], pb[0:64, :], AF.Copy, scale=0.125)
            b.ts(qaug[1][0:64, ts_], pb[64:128, :], 0.125, None, ALU.mult)
            pb = banks[1]
            for kc in range(8):
                b.mm(pb[:, :], wk[:, kc, cs], hT[:, kc, ts_], kc == 0, kc == 7)
            b.act(kaug[0][0:64, ts_], pb[0:64, :], AF.Copy)
            b.copy(kaug[1][0:64, ts_], pb[64:128, :])
            for j in range(2):
                b.rsum(ksum[0:64, 16 * j + 2 * tc:16 * j + 2 * tc + 2],
                       pb[64 * j:64 * j + 64, :].rearrange("p (n s) -> p n s", s=256))
            pb = banks[2 + tc % 2]
            for kc in range(8):
                b.mm(pb[:, :], wz[:, kc, cs], hT[:, kc, ts_], kc == 0, kc == 7)
            b.act(thz[tc % 2][:, :], pb[:, :], AF.Tanh, scale=0.5)
            b.stt(zs[:, ts_], thz[tc % 2][:, :], 1.0, pb[:, :], ALU.add, ALU.mult)
        b.ts(kbarT[0][:, :], ksum[:, 0:16], 1.0 / 256, None, ALU.mult)
        b.ts(kbarT[1][:, :], ksum[:, 16:32], 1.0 / 256, None, ALU.mult)
        for j in range(2):
            for tt_ in range(NT):
                b.mm(gb_[:, tt_ * 16:(tt_ + 1) * 16], qaug[j][0:64, tt_ * 128:(tt_ + 1) * 128], kbarT[j][:, :])
            b.tt(Gs[:, :], gb_[:, :], pastb[:, :], ALU.add)
            for tt_ in range(NT):
                b.max8(top8[:, tt_, :], Gs[:, tt_ * 16:(tt_ + 1) * 16])
            mb = mbias2[j]
            b.tt(mb[:, :].rearrange("p (t n) -> p t n", n=16), Gs[:, :].rearrange("p (t n) -> p t n", n=16),
                 bc_last(top8[:, :, 2], 16), ALU.is_lt)
            b.ts(mb[:, :], mb[:, :], NEG, None, ALU.mult)
            own = bass.AP(mb, 0, [[512, 128], [33, 16], [16, 2]])
            b.memset(own, 0.0)
        for g in range(8):
            pb = banks[4 + g % 2]
            for i in range(4):
                tt_ = g * 4 + i
                for kc in range(8):
                    b.mm(pb[:, i * 128:(i + 1) * 128], hT[:, kc, tt_ * 128:(tt_ + 1) * 128], wv[:, kc, cs],
                         kc == 0, kc == 7)
            b.act(vP[:, g * 4:(g + 1) * 4, :, 0:64], pb[:, :].rearrange("p (t j c) -> p t j c", j=2, c=64), AF.Copy)
        if p == 3:
            for w, c0 in ((wq, 2048), (wz, 3584), (wk, 3072), (wv, 2560)):
                load_w(w, win_d, c0, 512, 8)
        for j in range(2):
            mb = mbias2[j]
            for g in range(8):
                for i in range(4):
                    tt_ = g * 4 + i
                    b.tr(tb_[0:16, i * 128:(i + 1) * 128], mb[:, tt_ * 16:(tt_ + 1) * 16], identf[:])
                b.copy(qaug[j][64:80, g * 512:(g + 1) * 512], tb_[0:16, :])
        for j in range(2):
            q_, k_ = qaug[j], kaug[j]
            iters = [(tc, st) for tc in range(8) for st in range(4 * tc + 4)]

            def qk(n):
                tc, st = iters[n]
                dj = st - 4 * tc
                c0 = 128 * dj if dj >= 0 else 0
                Sb = sbk[n % 4]
                kl = k_[0:84, st * 128:(st + 1) * 128]
                if dj < 0:
                    b.mm(Sb[:, :], kl, q_[0:84, tc * 512:(tc + 1) * 512])
                else:
                    b.mm(Sb[:, c0:c0 + 128], identb[:], trib[:], True, False)
                    b.mm(Sb[:, c0:c0 + 128], kl, q_[0:84, tc * 512 + c0:tc * 512 + c0 + 128], False, True)
                    if c0 + 128 < 512:
                        b.mm(Sb[:, c0 + 128:512], kl, q_[0:84, tc * 512 + c0 + 128:(tc + 1) * 512])

            qk(0)
            qk(1)
            for n, (tc, st) in enumerate(iters):
                if n + 2 < len(iters):
                    qk(n + 2)
                dj = st - 4 * tc
                c0 = 128 * dj if dj >= 0 else 0
                nst = 4 * tc + 4
                O = ob[tc % 2]
                b.act(PT[n % 4][:, c0:512], sbk[n % 4][:, c0:512], AF.Exp)
                b.mm(O[:, c0:512], vP[:, st, j, :], PT[n % 4][:, c0:512], st == 0, st == nst - 1)
                if st == nst - 1:
                    ts_ = slice(tc * 512, (tc + 1) * 512)
                    rr = slice(64 * j, 64 * j + 64)
                    b.recip(rec[0:64, :], O[64:128, :])
                    b.tt(tmpn[rr, :], O[0:64, :], rec[0:64, :], ALU.mult)
                    b.stt(yaT[rr, ts_], tmpn[rr, :], 0.5, zs[rr, ts_], ALU.mult, ALU.mult)
        b.dma("sp", ya_d[p * 128:(p + 1) * 128, :], yaT[:, :])

    S.barrier()
    b.sb_off = a_mark
    wf, wg, wqh, wi = wq, wz, wk, wv
    TOP = 188352
    wqc = b.sb("wqc", [128, 8, 512], BF16, off=TOP)
    wzc = b.sb("wzc", [128, 8, 512], BF16, off=TOP + 8192)
    wa = b.sb("wa", [128, 4, 1024], BF16, off=TOP + 16384)
    wb = b.sb("wb", [128, 4, 1024], BF16, off=TOP + 24576)
    wc = b.sb("wc", [128, 4, 1024], BF16, off=TOP + 32768)
    load_w(wqc, win_d, 4096, 512, 8)
    load_w(wzc, win_d, 4608, 512, 8)
    load_w(wa, wa_d, 0, 1024, 4)
    load_w(wb, wb_d, 0, 1024, 4)
    load_w(wc, wc_d, 0, 1024, 4)
    lbl = b.sb("lbl", [128, 8], F32)
    lb = b.sb("lb", [128, 4], F32)
    homl = b.sb("homl", [128, 4], F32)
    nhoml = b.sb("nhoml", [128, 4], F32)
    lnb = b.sb("lnb", [128, 4], F32)
    hnw = b.sb("hnw", [128, 1], F32)
    hnwh = b.sb("hnwh", [128, 1], F32)
    trim = b.sb("trim", [128, 128], F32)
    rmask = b.sb("rmask", [128, 512], F32)
    b.dma("sp", lbl[:], lbl_d)
    b.dma("sp", hnw[:], hnw_d)
    b.dma("sp", trim[:], trim_d)
    b.dma("sp", rmask[:], rmask_d)
    b.tt(lb[:, :], lbl[:, 0:4], lbl[:, 4:8], ALU.subtract)
    b.act(lb[:, :], lb[:, :], AF.Tanh, scale=0.5)
    b.ts(lb[:, :], lb[:, :], 0.5, 0.5, ALU.mult, ALU.add)
    b.ts(homl[:, :], lb[:, :], -0.5, 0.5, ALU.mult, ALU.add)
    b.ts(nhoml[:, :], homl[:, :], -1.0, None, ALU.mult)
    b.tt(lnb[:, :], homl[:, :], lb[:, :], ALU.add)
    b.ts(hnwh[:, :], hnw[:, :], 0.5, None, ALU.mult)

    def two(name, shape, dt):
        return [b.sb(f"{name}{i}", shape, dt) for i in range(2)]
    thf = [b.sb("thf", [128, 512], F32)] * 2
    thg = [b.sb("thg", [128, 512], F32)] * 2
    gs2 = two("gs2", [128, 512], F32)
    kk = two("kk", [128, 512], F32)
    lf = [b.sb("lf", [128, 512], F32)] * 2
    bb = two("bb", [128, 512], F32)
    eb = two("eb", [128, 512], F32)
    enb = two("enb", [128, 512], F32)
    edl = two("edl", [128, 512], F32)
    qt = two("qt", [128, 512], BF16)
    qsb = two("qsb", [128, 512], F32)
    kt = two("kt", [128, 512], BF16)
    kdT = two("kdT", [128, 512], BF16)
    vtm = two("vtm", [128, 512], BF16)
    kd = b.sb("kd", [128, 512], BF16)
    ATm = b.sb("ATm", [128, 512], BF16)
    Sst = b.sb("Sst", [128, 128], F32)
    SbfA = two("SbfA", [128, 4, 128], BF16)
    sq = b.sb("sq", [128, 512], BF16)
    rl = b.sb("rl", [128, 512], F32)
    rs2 = b.sb("rs2", [128, 512], F32)
    obn = b.sb("obn", [128, 512], F32)
    ybT = b.sb("ybT", [128, T], BF16)
    assert b.sb_off <= TOP, b.sb_off
    pf_, pq_, pg_, pv_, pAT, pU, po_, px = banks
    its = [(hh, tc) for hh in range(4) for tc in range(8)]

    def stage1_pe(it, part):
        hh, tc = its[it]
        cs = slice(hh * 128, (hh + 1) * 128)
        ts_ = slice(tc * 512, (tc + 1) * 512)
        if part == 0:
            for kc in range(8):
                b.mm(pf_[:, :], wf[:, kc, cs], hT[:, kc, ts_], kc == 0, kc == 7)
        elif part == 1:
            for kc in range(8):
                b.mm(pg_[:, :], wg[:, kc, cs], hT[:, kc, ts_], kc == 0, kc == 7)
        elif part == 2:
            for kc in range(8):
                b.mm(pq_[:, :], wqh[:, kc, cs], hT[:, kc, ts_], kc == 0, kc == 7)
        else:
            for i in range(4):
                tt_ = tc * 4 + i
                for kc in range(8):
                    b.mm(pv_[:, i * 128:(i + 1) * 128], hT[:, kc, tt_ * 128:(tt_ + 1) * 128], wi[:, kc, cs],
                         kc == 0, kc == 7)

    def stage1_early(it, part=None):
        hh, tc = its[it]
        s_ = it % 2
        if part in (None, 0):
            b.act(thf[s_][:, :], pf_[:, :], AF.Tanh, scale=0.5)
            b.act(thg[s_][:, :], pg_[:, :], AF.Tanh, scale=0.5)
        if part in (None, 1):
            b.act(vtm[s_][:, :], pv_[:, :], AF.Copy)
            b.stt(gs2[s_][:, :], thg[s_][:, :], 1.0, pg_[:, :], ALU.add, ALU.mult)
        if part in (None, 2):
            b.copy(qsb[s_][:, :], pq_[:, :])

    def stage1_ew(it):
        hh, tc = its[it]
        s_ = it % 2
        b.ts(kk[s_][:, :], thf[s_][:, :], nhoml[:, hh:hh + 1], homl[:, hh:hh + 1], ALU.mult, ALU.add)
        b.act(lf[s_][:, :], thf[s_][:, :], AF.Ln, bias=lnb[:, hh:hh + 1], scale=homl[:, hh:hh + 1])
        b.scan(bb[s_][:, :], rmask[:, :], lf[s_][:, :], 0.0, ALU.mult, ALU.add)
        b.act(eb[s_][:, :], bb[s_][:, :], AF.Exp)
        b.act(enb[s_][:, :], bb[s_][:, :], AF.Exp, scale=-1.0)
        for c in range(4):
            b.act(edl[s_][:, c * 128:(c + 1) * 128], bb[s_][:, c * 128:(c + 1) * 128], AF.Exp,
                  bias=bb[s_][:, c * 128 + 127:c * 128 + 128], scale=-1.0)
        b.tt(qt[s_][:, :], qsb[s_][:, :], eb[s_][:, :], ALU.mult)
        b.tt(kt[s_][:, :], kk[s_][:, :], enb[s_][:, :], ALU.mult)
        b.tt(kdT[s_][:, :], kk[s_][:, :], edl[s_][:, :], ALU.mult)

    def stage2(it, part):
        hh, tc = its[it]
        s_ = it % 2
        pxb = px[:].bitcast(BF16)
        if part == 0:
            if tc == 0:
                b.memset(Sst[:], 0.0)
                b.memset(SbfA[s_][:, 0, :], 0.0)
            for c in range(4):
                b.tr(pxb[:, c * 128:(c + 1) * 128], kdT[s_][:, c * 128:(c + 1) * 128], identb[:])
            b.copy(kd[:, :], pxb[:, 0:512])
            for c in range(4):
                cc = slice(c * 128, (c + 1) * 128)
                b.mm(pAT[:, cc], kt[s_][:, cc], qt[s_][:, cc])
            trim4 = bass.AP(trim, 0, [[128, 128], [0, 4], [1, 128]])
            b.tt(ATm[:, :].rearrange("p (c t) -> p c t", t=128), pAT[:, :].rearrange("p (c t) -> p c t", t=128),
                 trim4, ALU.mult)
        elif part == 1:
            for c in range(4):
                cc = slice(c * 128, (c + 1) * 128)
                b.mm(pU[:, cc], kd[:, cc], vtm[s_][:, cc])
            for c in range(4):
                cc = slice(c * 128, (c + 1) * 128)
                b.stt(Sst[:, :], Sst[:, :], eb[s_][:, c * 128 + 127:c * 128 + 128], pU[:, cc], ALU.mult, ALU.add)
                dst = SbfA[s_][:, c + 1, :] if c < 3 else SbfA[1 - s_][:, 0, :]
                b.copy(dst, Sst[:, :])
        else:
            for c in range(4):
                cc = slice(c * 128, (c + 1) * 128)
                b.mm(po_[:, cc], vtm[s_][:, cc], ATm[:, cc], True, False)
                b.mm(po_[:, cc], SbfA[s_][:, c, :], qt[s_][:, cc], False, True)
            b.act(sq[:, :], po_[:, :], AF.Square)

    def stage2b(it):
        hh, tc = its[it]
        s_ = it % 2
        ts_ = slice(tc * 512, (tc + 1) * 512)
        b.mm(px[:, :], onesb[:, :], sq[:, :])
        b.act(rl[:, :], px[:, :], AF.Ln, bias=epsb[:, 0:1], scale=1.0 / 128)
        b.act(rs2[:, :], rl[:, :], AF.Exp, scale=-0.5)
        b.tt(obn[:, :], po_[:, :], rs2[:, :], ALU.mult)
        b.stt(ybT[:, ts_], obn[:, :], hnwh[:, 0:1], gs2[s_][:, :], ALU.mult, ALU.mult)
        if tc == 7:
            b.dma("sp", yb_d[hh * 128:(hh + 1) * 128, :], ybT[:, :])

    for part in range(4):
        stage1_pe(0, part)
    stage1_early(0)
    stage1_ew(0)
    for it in range(len(its)):
        nxt = it + 1 < len(its)
        if HGRN_FINE:
            if nxt:
                stage1_pe(it + 1, 0)
            stage2(it, 0)
            if nxt:
                stage1_pe(it + 1, 1)
            if it > 0:
                stage2b(it - 1)
            stage2(it, 1)
            if nxt:
                stage1_pe(it + 1, 2)
            stage2(it, 2)
            if nxt:
                stage1_pe(it + 1, 3)
                stage1_early(it + 1)
                stage1_ew(it + 1)
        else:
            if nxt:
                for part in range(2):
                    stage1_pe(it + 1, part)
            if it > 0:
                stage2b(it - 1)
            if nxt:
                stage1_early(it + 1, 0)
            stage2(it, 0)
            if nxt:
                stage1_pe(it + 1, 3)
                stage1_early(it + 1, 1)
            stage2(it, 1)
            if nxt:
                stage1_pe(it + 1, 2)
                stage1_early(it + 1, 2)
            stage2(it, 2)
            if nxt:
                stage1_ew(it + 1)
    wga = hT[:, 0:2, :].rearrange("p a (b c) -> p (a b) c", c=1024)
    wgb = hT[:, 2:4, :].rearrange("p a (b c) -> p (a b) c", c=1024)
    wgc = hT[:, 4:6, :].rearrange("p a (b c) -> p (a b) c", c=1024)
    wo = hT[:, 6:8, :].rearrange("p a (b c) -> p (a b) c", c=1024)
    for i, w in enumerate((wga, wgb, wgc)):
        load_w(w, win_d, 5120 + i * 1024, 1024, 8)
    load_w(wo, wo_d, 0, 1024, 8)
    stage2b(len(its) - 1)

    S.barrier()
    b.sb_off = ph_mark
    postw = b.sb("postw", [128, D], F32)
    b.dma("sp", postw[:], postw_d)
    eps4 = b.sb("eps4", [128, 1], F32)
    b.memset(eps4[:], 4.0 * EPS)
    hTc = b.sb("hTc", [128, 8, 512], BF16)
    xn = [b.sb(f"xn{i}", [128, D], F32) for i in range(3)]
    xr = [b.sb(f"xr{i}", [128, D], F32) for i in range(2)]
    hbN = [b.sb(f"hbN{i}", [128, D], BF16) for i in range(4)]
    junk = b.sb("junk2", [128, 512], BF16)
    ssn = b.sb("ssn", [128, 4], F32)
    rstdn = b.sb("rstdn", [128, 4], F32)
    mhalf = b.sb("mhalf", [128, 1], F32)
    b.memset(mhalf[:], -0.5)
    ss2 = two("ss3", [128, 2], F32)
    rstd2 = two("rstd3", [128, 1], F32)
    yac = b.sb("yac", [128, 4, 512], BF16)
    ybc = b.sb("ybc", [128, 4, 512], BF16)
    ycc = b.sb("ycc", [128, 4, 512], BF16)
    qcT = [b.sb(f"qcT{i}", [128, 512], BF16) for i in range(4)]
    thc = two("thc", [128, 512], F32)
    zs2 = [b.sb(f"zs2c{i}", [128, 512], F32) for i in range(4)]
    PX = [b.sb(f"PX{i}", [128, 512], BF16) for i in range(4)]
    recx = two("recx", [128, 512], F32)
    tmpx2 = [m1, m2] = two("tmpx", [128, 512], F32)
    sgs = [b.sb(f"sgs{i}", [128, 512], F32) for i in range(4)]
    mT = b.sb("mT", [128, 8, 512], BF16)
    tmpo = recx[0]
    assert b.sb_off <= TOP, b.sb_off
    yav = ya_d.rearrange("(k p) t -> p k t", p=128)
    ybv = yb_d.rearrange("(k p) t -> p k t", p=128)
    gcount = 0
    xn_i = [0]
    xn_of = {}

    def nA_load(tc, i):
        k = xn_i[0] % 3
        xn_i[0] += 1
        xn_of[(tc, i)] = xn[k]
        r1 = tc * 512 + i * 128
        b.dma("sp", xn[k][:], x_d[r1:r1 + 128, :])

    def nA_stats(tc, i):
        xt = xn_of[(tc, i)]
        b.act(hbN[i][:], xt[:], AF.Square, accum_out=ssn[:, i:i + 1])
        b.ts(rstdn[:, i:i + 1], ssn[:, i:i + 1], 1.0 / D, EPS, ALU.mult, ALU.add, eng="pool")
        b.tt(rstdn[:, i:i + 1], rstdn[:, i:i + 1], mhalf[:, 0:1], ALU.pow, eng="pool")

    def nA_scale(tc, i):
        xt = xn_of[(tc, i)]
        b.act(hbN[i][:], xt[:], AF.Copy, scale=rstdn[:, i:i + 1])

    def normB(i):
        tpv = banks[6 + i % 2][:].bitcast(BF16)
        for kc in range(8):
            b.tr(tpv[:, kc * 128:(kc + 1) * 128], hbN[i][:, kc * 128:(kc + 1) * 128], identb[:])
        b.tt(hTc[:, :, i * 128:(i + 1) * 128], tpv.rearrange("p (k t) -> p k t", t=128),
             bc_last(prew[:, :], 128), ALU.mult)

    def normA_steps(tc):
        return [
            lambda: (nA_load(tc, 0), nA_load(tc, 1), nA_load(tc, 2), nA_stats(tc, 0)),
            lambda: (nA_stats(tc, 1), nA_scale(tc, 0), nA_load(tc, 3)),
            lambda: (nA_stats(tc, 2), nA_scale(tc, 1)),
            lambda: (nA_stats(tc, 3), nA_scale(tc, 2)),
            lambda: (nA_scale(tc, 3),),
        ]

    def xr_load(tc, i):
        r1 = tc * 512 + i * 128
        b.dma("sp", xr[i % 2][:], x_d[r1:r1 + 128, :])

    for st_ in normA_steps(0):
        st_()
    b.dma("sp", yac[:, :, :], yav[:, :, 0:512])
    b.dma("sp", ybc[:, :, :], ybv[:, :, 0:512])
    for i in range(4):
        normB(i)
    for tc in range(8):
        ts_ = slice(tc * 512, (tc + 1) * 512)
        xr_load(tc, 0)
        xr_load(tc, 1)
        for h in range(4):
            cs = slice(h * 128, (h + 1) * 128)
            for kc in range(8):
                b.mm(banks[h][:, :], wqc[:, kc, cs], hTc[:, kc, :], kc == 0, kc == 7)
            b.act(qcT[h][:, :], banks[h][:, :], AF.Copy, scale=float(128 ** -0.5))

        def zproj(h, bank):
            cs = slice(h * 128, (h + 1) * 128)
            for kc in range(8):
                b.mm(bank[:, :], wzc[:, kc, cs], hTc[:, kc, :], kc == 0, kc == 7)
            b.act(thc[h % 2][:, :], bank[:, :], AF.Tanh, scale=0.5)
            b.stt(zs2[h][:, :], thc[h % 2][:, :], 1.0, bank[:, :], ALU.add, ALU.mult)

        def lmm(h0):
            for hh_ in (h0, h0 + 1):
                for mt in range(2):
                    k = 2 * (hh_ - h0) + mt
                    b.mm(banks[k][:, :], kmT[:, hh_, mt * 128:(mt + 1) * 128], qcT[hh_][:, :])
                    b.act(PX[k][:, :], banks[k][:, :], AF.Exp)

        def pvn(h0):
            for hh_ in (h0, h0 + 1):
                cs = slice(hh_ * 128, (hh_ + 1) * 128)
                O = banks[4 + 2 * (hh_ - h0)]
                Dn = banks[5 + 2 * (hh_ - h0)]
                for mt in range(2):
                    b.mm(O[:, :], vm[:, mt, cs], PX[2 * (hh_ - h0) + mt][:, :], mt == 0, mt == 1)
                for mt in range(2):
                    b.mm(Dn[:, :], onesb[:, :], PX[2 * (hh_ - h0) + mt][:, :], mt == 0, mt == 1)
            for hh_ in (h0, h0 + 1):
                O = banks[4 + 2 * (hh_ - h0)]
                Dn = banks[5 + 2 * (hh_ - h0)]
                tx = tmpx2[hh_ - h0]
                b.act(tx[:, :], Dn[:, :], AF.Ln)
                b.act(recx[hh_ - h0][:, :], tx[:, :], AF.Exp, scale=-1.0)
                b.tt(tx[:, :], O[:, :], recx[hh_ - h0][:, :], ALU.mult)
                b.stt(ycc[:, hh_, :], tx[:, :], 0.5, zs2[hh_][:, :], ALU.mult, ALU.mult)

        zproj(0, banks[4])
        zproj(1, banks[5])
        lmm(0)
        zproj(2, banks[6])
        zproj(3, banks[7])
        pvn(0)
        lmm(2)
        pvn(2)
        nsteps = normA_steps(tc + 1) if tc + 1 < 8 else []
        for oc in range(8):
            os_ = slice(oc * 128, (oc + 1) * 128)
            Pbs = []
            sg_ = []
            for bi, (wgx, wx, yx) in enumerate(((wga, wa, yac), (wgb, wb, ybc), (wgc, wc, ycc))):
                G = banks[gcount % 2]
                Pb = banks[2 + gcount % 4]
                sgb = sgs[gcount % 4]
                gcount += 1
                for kc in range(8):
                    b.mm(G[:, :], wgx[:, kc, os_], hTc[:, kc, :], kc == 0, kc == 7)
                b.act(sgb[:, :], G[:, :], AF.Tanh, scale=0.5)
                for k4 in range(4):
                    b.mm(Pb[:, :], wx[:, k4, os_], yx[:, k4, :], k4 == 0, k4 == 3)
                Pbs.append(Pb)
                sg_.append(sgb)
            b.stt(m1[:, :], sg_[0][:, :], 1.0, Pbs[0][:, :], ALU.add, ALU.mult)
            b.stt(m2[:, :], sg_[1][:, :], 1.0, Pbs[1][:, :], ALU.add, ALU.mult)
            b.tt(m1[:, :], m1[:, :], m2[:, :], ALU.add)
            b.stt(m2[:, :], sg_[2][:, :], 1.0, Pbs[2][:, :], ALU.add, ALU.mult)
            b.tt(mT[:, oc, :], m1[:, :], m2[:, :], ALU.add)
            if 1 <= oc <= 5 and nsteps:
                nsteps[oc - 1]()
        if tc + 1 < 8:
            tn_ = slice((tc + 1) * 512, (tc + 2) * 512)
            b.dma("sp", yac[:, :, :], yav[:, :, tn_])
            b.dma("sp", ybc[:, :, :], ybv[:, :, tn_])
        for i in range(4):
            xt = xr[i % 2]
            r0 = tc * 512 + i * 128
            Y = [banks[(2 * i) % 4], banks[(2 * i) % 4 + 1]]
            s2 = ss2[i % 2]
            r2 = rstd2[i % 2]
            for hf in range(2):
                for kc in range(8):
                    b.mm(Y[hf][:, :], mT[:, kc, i * 128:(i + 1) * 128], wo[:, kc, hf * 512:(hf + 1) * 512],
                         kc == 0, kc == 7)
                b.act(junk[:, :], Y[hf][:, :], AF.Square, accum_out=s2[:, hf:hf + 1])
            if tc + 1 < 8:
                normB(i)
            b.tt(r2[:, 0:1], s2[:, 0:1], s2[:, 1:2], ALU.add, eng="pool")
            b.ts(r2[:, 0:1], r2[:, 0:1], 1.0 / D, 4.0 * EPS, ALU.mult, ALU.add, eng="pool")
            b.tt(r2[:, 0:1], r2[:, 0:1], mhalf[:, 0:1], ALU.pow, eng="pool")
            for hf in range(2):
                b.stt(tmpo[:, :], Y[hf][:, :], r2[:, 0:1], postw[:, hf * 512:(hf + 1) * 512], ALU.mult, ALU.mult)
                b.tt(xt[:, hf * 512:(hf + 1) * 512], xt[:, hf * 512:(hf + 1) * 512], tmpo[:, :], ALU.add)
            b.dma("sp", out_d[r0:r0 + 128, :], xt[:])
            if i + 2 < 4:
                xr_load(tc, i + 2)
    S.final_wait()

    from contextlib import ExitStack
    with ExitStack() as es:
        sems = {e: es.enter_context(nc.semaphore(f"s_{e}")) for e in Sched.CE}
        dma_sems = {
            "sp": [es.enter_context(nc.semaphore(f"d_sp{i}")) for i in range(24)],
            "pool": [es.enter_context(nc.semaphore(f"d_pl{i}")) for i in range(16)],
        }
        block = es.enter_context(nc.Block())
        S.emit(block, sems, dma_sems)
    return nc


def _consts():
    bf = ml_dtypes.bfloat16
    c = {}
    c["identb"] = np.eye(128, dtype=np.float32).astype(bf)
    c["identf"] = np.eye(128, dtype=np.float32)
    s = np.arange(128)[:, None]
    t = np.arange(128)[None, :]
    c["trib"] = np.where(s <= t, 0.0, NEG).astype(np.float32).astype(bf)
    c["trim"] = (s <= t).astype(np.float32)
    pos = np.arange(T)
    kac = np.zeros((20, T), np.float32)
    for n in range(16):
        kac[n] = (pos // 256 == n)
    kac[16] = pos % 128
    kac[17] = (pos // 128) * 128
    kac[18] = 1.0
    kac[19] = 1.0
    c["kac"] = kac.astype(bf)
    qac = np.zeros((8, 4, T), np.float32)
    for h in range(8):
        slope = 2.0 ** (-(h + 1))
        qac[h, 0] = slope
        qac[h, 1] = slope
        qac[h, 2] = -slope * (pos % 128)
        qac[h, 3] = -slope * ((pos // 128) * 128)
    c["qac"] = qac.astype(bf)
    pastb = np.zeros((128, 32, 16), np.float32)
    for tt in range(32):
        pastb[:, tt, tt // 2:] = -1e30
    c["pastb"] = pastb.reshape(128, 512)
    rm = np.ones((128, 512), np.float32)
    rm[:, ::128] = 0.0
    c["rmask"] = rm
    return c


_NC = [None]


def kernel(x, mem, pre_norm_w, w_in, hgrn_lb_logits, hgrn_norm_w, mem_norm_w, w_mem_kv,
           w_branch_a, w_branch_b, w_branch_c, w_out, post_norm_w):
    f = lambda a: np.ascontiguousarray(np.asarray(a, dtype=np.float32))
    x = f(x)
    mem = f(mem)
    if _NC[0] is None:
        _NC[0] = build()
    nc = _NC[0]
    shared = dict(_consts())
    shared["w_in"] = f(w_in)[0]
    shared["w_kv"] = f(w_mem_kv)[0]
    shared["w_a"] = f(w_branch_a)[0]
    shared["w_b"] = f(w_branch_b)[0]
    shared["w_c"] = f(w_branch_c)[0]
    shared["w_o"] = f(w_out)[0]
    shared["prew"] = np.ascontiguousarray(f(pre_norm_w)[0].reshape(8, 128).T)
    shared["memw"] = np.ascontiguousarray(f(mem_norm_w)[0].reshape(8, 128).T)
    shared["postw"] = np.ascontiguousarray(np.broadcast_to(f(post_norm_w)[0][None, :], (128, D)))
    lbl = f(hgrn_lb_logits).reshape(2, 4, 128)
    shared["lbl"] = np.ascontiguousarray(lbl.transpose(2, 0, 1).reshape(128, 8))
    shared["hnw"] = np.ascontiguousarray(f(hgrn_norm_w)[0].reshape(128, 1))
    in_maps = []
    for c in range(8):
        m = dict(shared)
        m["x"] = x[c]
        m["mem"] = mem[c]
        in_maps.append(m)
    res = run_bass_kernel_spmd(nc, in_maps, core_ids=list(range(8)))
    kernel.last = res
    return np.stack([r["out"] for r in res.results], axis=0).astype(np.float32)
```

```python
import numpy as np
import ml_dtypes
import concourse.bass as bass
import concourse.mybir as mybir
from concourse.bass_utils import run_bass_kernel_spmd

F32 = mybir.dt.float32
BF16 = mybir.dt.bfloat16
AF = mybir.ActivationFunctionType
ALU = mybir.AluOpType
AX = mybir.AxisListType

T = 4096
D = 1024
NT = T // 128
NEG = -30000.0
EPS = 1e-6
DEBUG = False
HGRN_FINE = False


def _es(dt):
    return mybir.dt.size(dt)


class Op:
    __slots__ = ("eng", "fn", "deps", "dma", "sig", "cnt", "sem", "target", "waited", "prev_target")

    def __init__(self, eng, fn, deps, dma):
        self.eng = eng
        self.fn = fn
        self.deps = deps
        self.dma = dma
        self.sig = False
        self.cnt = 0
        self.sem = None
        self.target = 0
        self.waited = False


class Sched:
    CE = ("pe", "act", "dve", "pool")

    def __init__(self, nc):
        self.nc = nc
        self.ops = []
        self.kinds = {}
        self.recs = {}
        self.last = {}
        self.barrier_deps = {}
        self.open_dmas = set()

    def reg_tensor(self, handle, kind):
        self.kinds[handle.name] = kind
        return handle

    def region(self, ap):
        name = ap.tensor.name
        kind = self.kinds[name]
        es = _es(ap.dtype)
        pat = list(ap.ap)
        off = int(ap.offset)
        if kind == "dram":
            ext = 1 + sum((c - 1) * abs(s) for s, c in pat)
            return (name, 0, 1, off * es, (off + ext) * es)
        pstep, npart = pat[0]
        p0 = off // pstep
        f0 = off % pstep
        ext = 1 + sum((c - 1) * abs(s) for s, c in pat[1:])
        if kind == "ps":
            return (name, 0, 128, 0, 2048)
        return (name, p0, p0 + npart, f0 * es, (f0 + ext) * es)

    def _scan(self, reg, is_write, eng, idx, deps):
        name, p0, p1, b0, b1 = reg
        lst = self.recs.setdefault(name, [])
        keep = []
        for r in lst:
            rp0, rp1, rb0, rb1, rop, reng, rw = r
            ov = rp0 < p1 and p0 < rp1 and rb0 < b1 and b0 < rb1
            if ov:
                if is_write:
                    deps.append((rop, "WAW" if rw else "WAR"))
                elif rw:
                    deps.append((rop, "RAW"))
            if is_write and ov and p0 <= rp0 and rp1 <= p1 and b0 <= rb0 and rb1 <= b1:
                continue
            if (not is_write) and (not rw) and reng == eng and (rp0, rp1, rb0, rb1) == (p0, p1, b0, b1) \
                    and not self.ops[rop].dma:
                continue
            keep.append(r)
        keep.append((p0, p1, b0, b1, idx, eng, is_write))
        self.recs[name] = keep

    def add(self, eng, fn, reads=(), writes=(), dma=False):
        idx = len(self.ops)
        deps = []
        for a in reads:
            self._scan(self.region(a), False, eng, idx, deps)
        for a in writes:
            self._scan(self.region(a), True, eng, idx, deps)
        if eng in self.barrier_deps:
            for d in self.barrier_deps.pop(eng):
                deps.append((d, "RAW"))
        op = Op(eng, fn, deps, dma)
        self.ops.append(op)
        for d, _k in deps:
            if self.ops[d].dma:
                self.ops[d].waited = True
                self.open_dmas.discard(d)
        if dma:
            self.open_dmas.add(idx)
        else:
            self.last[eng] = idx
        return idx

    def barrier(self):
        deps = set(self.last.values()) | set(self.open_dmas)
        for e in ("pe", "act", "dve", "pool", "sp"):
            self.barrier_deps[e] = set(deps)
        self.recs = {}

    def final_wait(self):
        self.barrier()
        self.add("sp", None)

    def emit(self, block, sems, dma_sems):
        ops = self.ops
        def real(o, d, kind):
            od = ops[d]
            if od is o:
                return False
            if od.dma or o.dma:
                return True
            if od.eng == o.eng and o.eng == "pe":
                return False
            return True

        for o in ops:
            nd = {}
            for d, kind in o.deps:
                if real(o, d, kind):
                    nd[d] = True
            o.deps = sorted(nd.keys())
            for d in o.deps:
                ops[d].sig = True
        cnt = {e: 0 for e in self.CE}
        rr = {q: 0 for q in dma_sems}
        tot = {}
        for o in ops:
            if o.dma:
                pool = dma_sems[o.eng]
                s = pool[rr[o.eng] % len(pool)]
                rr[o.eng] += 1
                o.sem = s
                o.prev_target = tot.get(id(s), 0)
                tot[id(s)] = o.prev_target + 16
                o.target = tot[id(s)]
            elif o.sig and o.fn is not None:
                cnt[o.eng] += 1
                o.cnt = cnt[o.eng]
        per_eng = {e: [] for e in ("pe", "act", "dve", "pool", "sp")}
        for o in ops:
            per_eng[o.eng].append(o)

        def run(e, eh):
            waited_c = {x: 0 for x in self.CE}
            waited_d = {}
            for o in per_eng[e]:
                need_c = {}
                need_d = {}
                for d in o.deps:
                    od = ops[d]
                    if od.dma:
                        k = id(od.sem)
                        if need_d.get(k, (None, 0))[1] < od.target:
                            need_d[k] = (od.sem, od.target)
                    else:
                        if need_c.get(od.eng, 0) < od.cnt:
                            need_c[od.eng] = od.cnt
                if o.dma and o.prev_target > 0:
                    k = id(o.sem)
                    if need_d.get(k, (None, 0))[1] < o.prev_target:
                        need_d[k] = (o.sem, o.prev_target)
                for x, v in need_c.items():
                    if v > waited_c[x]:
                        eh.wait_ge(sems[x], v)
                        waited_c[x] = v
                for k, (s, v) in need_d.items():
                    if v > waited_d.get(k, 0):
                        eh.wait_ge(s, v)
                        waited_d[k] = v
                if o.fn is None:
                    continue
                inst = o.fn(eh)
                if o.dma:
                    inst.then_inc(o.sem, 16)
                elif o.sig:
                    inst.then_inc(sems[e], 1)

        @block.tensor
        def _(eh):
            run("pe", eh)

        @block.scalar
        def _(eh):
            run("act", eh)

        @block.vector
        def _(eh):
            run("dve", eh)

        @block.gpsimd
        def _(eh):
            run("pool", eh)

        @block.sync
        def _(eh):
            run("sp", eh)


class B:
    def __init__(self, nc):
        self.nc = nc
        self.S = Sched(nc)
        self.sb_off = 16512
        self.uid = 0

    def sb(self, name, shape, dt, off=None):
        nbytes = int(np.prod(shape[1:])) * _es(dt)
        if off is None:
            off = (self.sb_off + 63) // 64 * 64
            self.sb_off = off + nbytes
        assert off + nbytes <= 229376, (name, off, nbytes)
        self.uid += 1
        h = self.nc.alloc_sbuf_tensor_at(f"{name}_{self.uid}", list(shape), dt, offset=off)
        return self.S.reg_tensor(h, "sb")

    def ps(self, name):
        h = self.nc.alloc_psum_tensor(name, [128, 512], F32)
        return self.S.reg_tensor(h, "ps")

    def dram(self, name, shape, dt, kind):
        h = self.nc.dram_tensor(name, list(shape), dt, kind=kind)
        self.S.reg_tensor(h, "dram")
        return h.ap()

    def mm(self, out, lhsT, rhs, start=True, stop=True):
        rd = [lhsT, rhs] + ([] if start else [out])
        self.S.add("pe", lambda e: e.matmul(out, lhsT, rhs, start=start, stop=stop), rd, [out])

    def tr(self, out, in_, ident):
        self.S.add("pe", lambda e: e.transpose(out, in_, ident), [in_, ident], [out])

    def act(self, out, in_, func, bias=None, scale=None, accum_out=None):
        rd = [in_]
        kw = {}
        if bias is not None:
            kw["bias"] = bias
            if not isinstance(bias, (int, float)):
                rd.append(bias)
        if scale is not None:
            kw["scale"] = scale
            if not isinstance(scale, (int, float)):
                rd.append(scale)
        wr = [out]
        if accum_out is not None:
            kw["accum_out"] = accum_out
            wr.append(accum_out)
        self.S.add("act", lambda e: e.activation(out, in_, func, **kw), rd, wr)

    def tt(self, out, in0, in1, op, eng="dve"):
        self.S.add(eng, lambda e: e.tensor_tensor(out, in0, in1, op), [in0, in1], [out])

    def ts(self, out, in0, s1, s2, op0, op1=None, eng="dve"):
        rd = [in0]
        for s in (s1, s2):
            if s is not None and not isinstance(s, (int, float)):
                rd.append(s)
        if op1 is None:
            fn = lambda e: e.tensor_scalar(out, in0, s1, None, op0)
        else:
            fn = lambda e: e.tensor_scalar(out, in0, s1, s2, op0, op1)
        self.S.add(eng, fn, rd, [out])

    def stt(self, out, in0, scalar, in1, op0, op1, eng="dve"):
        rd = [in0, in1]
        if not isinstance(scalar, (int, float)):
            rd.append(scalar)
        self.S.add(eng, lambda e: e.scalar_tensor_tensor(out, in0, scalar, in1, op0, op1), rd, [out])

    def copy(self, out, in_, eng="dve"):
        self.S.add(eng, lambda e: e.tensor_copy(out, in_), [in_], [out])

    def memset(self, out, val, eng="dve"):
        self.S.add(eng, lambda e: e.memset(out, val), [], [out])

    def recip(self, out, in_):
        self.S.add("dve", lambda e: e.reciprocal(out, in_), [in_], [out])

    def rsum(self, out, in_):
        self.S.add("dve", lambda e: e.reduce_sum(out, in_, AX.X), [in_], [out])

    def max8(self, out, in_):
        self.S.add("dve", lambda e: e.max(out, in_), [in_], [out])

    def scan(self, out, d0, d1, init, op0, op1):
        self.S.add("dve", lambda e: e.tensor_tensor_scan(out, d0, d1, init, op0, op1), [d0, d1], [out])

    def dma(self, q, out, in_):
        self.S.add(q, lambda e: e.dma_start(out=out, in_=in_), [in_], [out], dma=True)


def bc_last(ap2, n):
    pat = list(ap2.ap)
    return bass.AP(ap2.tensor, ap2.offset, [list(p) for p in pat] + [[0, n]])


def build():
    nc = bass.Bass("TRN2", target_bir_lowering=False)
    b = B(nc)
    S = b.S
    x_d = b.dram("x", [T, D], F32, "ExternalInput")
    mem_d = b.dram("mem", [256, D], F32, "ExternalInput")
    win_d = b.dram("w_in", [D, 8192], F32, "ExternalInput")
    wkv_d = b.dram("w_kv", [D, 1024], F32, "ExternalInput")
    wa_d = b.dram("w_a", [512, D], F32, "ExternalInput")
    wb_d = b.dram("w_b", [512, D], F32, "ExternalInput")
    wc_d = b.dram("w_c", [512, D], F32, "ExternalInput")
    wo_d = b.dram("w_o", [D, D], F32, "ExternalInput")
    prew_d = b.dram("prew", [128, 8], F32, "ExternalInput")
    memw_d = b.dram("memw", [128, 8], F32, "ExternalInput")
    postw_d = b.dram("postw", [128, D], F32, "ExternalInput")
    lbl_d = b.dram("lbl", [128, 8], F32, "ExternalInput")
    hnw_d = b.dram("hnw", [128, 1], F32, "ExternalInput")
    identb_d = b.dram("identb", [128, 128], BF16, "ExternalInput")
    identf_d = b.dram("identf", [128, 128], F32, "ExternalInput")
    trib_d = b.dram("trib", [128, 128], BF16, "ExternalInput")
    trim_d = b.dram("trim", [128, 128], F32, "ExternalInput")
    kac_d = b.dram("kac", [20, T], BF16, "ExternalInput")
    qac_d = b.dram("qac", [8, 4, T], BF16, "ExternalInput")
    pastb_d = b.dram("pastb", [128, 512], F32, "ExternalInput")
    rmask_d = b.dram("rmask", [128, 512], F32, "ExternalInput")
    out_d = b.dram("out", [T, D], F32, "ExternalOutput")
    skind = "ExternalOutput" if DEBUG else "Internal"
    ya_d = b.dram("ya_s", [512, T], BF16, skind)
    yb_d = b.dram("yb_s", [512, T], BF16, skind)

    banks = [b.ps(f"bank{i}") for i in range(8)]

    identb = b.sb("identb", [128, 128], BF16)
    identf = b.sb("identf", [128, 128], F32)
    onesb = b.sb("onesb", [128, 128], BF16)
    prew = b.sb("prew", [128, 8], F32)
    memw = b.sb("memw", [128, 8], F32)
    kmT = b.sb("kmT", [128, 4, 256], BF16)
    vm = b.sb("vm", [128, 2, 512], BF16)
    for dst, src in ((identb, identb_d), (identf, identf_d), (prew, prew_d), (memw, memw_d)):
        b.dma("sp", dst[:], src)
    b.memset(onesb[:], 1.0)
    epsb = b.sb("epsb", [128, 1], F32)
    b.memset(epsb[:], EPS)
    persist_mark = b.sb_off

    xq = [None]

    def norm_tiles(src_d, row0, ntiles, wT, dstT, dst_col0, xts, hb, tp_bank, junk, ss, rstd):
        def stats(i):
            xt = xts[i % len(xts)]
            k = i % 4
            b.act(junk[:], xt[:], AF.Square, accum_out=ss[:, k:k + 1])
            b.act(rstd[:, k:k + 1], ss[:, k:k + 1], AF.Sqrt, bias=epsb[:, 0:1], scale=1.0 / D)
            b.recip(rstd[:, k:k + 1], rstd[:, k:k + 1])

        def mult(i):
            xt = xts[i % len(xts)]
            k = i % 4
            hbi = hb[i % 2]
            b.ts(hbi[:], xt[:], rstd[:, k:k + 1], None, ALU.mult)
            tpv = tp_bank[i % 2][:].bitcast(BF16)
            for kc in range(8):
                b.tr(tpv[:, kc * 128:(kc + 1) * 128], hbi[:, kc * 128:(kc + 1) * 128], identb[:])

        def evac(i):
            tpv = tp_bank[i % 2][:].bitcast(BF16)
            c0 = dst_col0 + i * 128
            b.tt(dstT[:, :, c0:c0 + 128], tpv.rearrange("p (k t) -> p k t", t=128),
                 bc_last(wT[:, :], 128), ALU.mult)

        def load(i):
            b.dma("sp", xts[i % len(xts)][:], src_d[row0 + i * 128: row0 + (i + 1) * 128, :])

        ahead = len(xts) - 2
        for i in range(min(ahead, ntiles)):
            load(i)
        stats(0)
        if ntiles > 1:
            stats(1)
        mult(0)
        for i in range(ntiles):
            if i + ahead < ntiles:
                load(i + ahead)
            if i + 2 < ntiles:
                stats(i + 2)
            if i + 1 < ntiles:
                mult(i + 1)
            evac(i)

    def load_w(dst, src_d, c0, ncols, nk):
        v = src_d.rearrange("(k p) c -> p k c", p=128)
        half = max(1, nk // 2)
        for k0 in range(0, nk, half):
            b.dma("pool", dst[:, k0:k0 + half, :], v[:, k0:k0 + half, c0:c0 + ncols])

    hT = b.sb("hT", [128, 8, T], BF16)
    ph_mark = b.sb_off
    wq = b.sb("wq", [128, 8, 512], BF16)
    wk = b.sb("wk", [128, 8, 512], BF16)
    wv = b.sb("wv", [128, 8, 512], BF16)
    wz = b.sb("wz", [128, 8, 512], BF16)
    a_mark = b.sb_off
    xts = [b.sb(f"xt{i}", [128, D], F32) for i in range(8)]
    hb = [b.sb(f"hb{i}", [128, D], BF16) for i in range(2)]
    junk = b.sb("junk", [128, D], BF16)
    ss = b.sb("ss", [128, 4], F32)
    rstd = b.sb("rstd", [128, 4], F32)
    memT = b.sb("memT", [128, 8, 256], BF16)
    wkv = b.sb("wkv", [128, 8, 1024], BF16)
    load_w(wkv, wkv_d, 0, 1024, 8)
    for i, w in enumerate((wq, wk, wv, wz)):
        load_w(w, win_d, i * 512, 512, 8)
    norm_tiles(mem_d, 0, 2, memw, memT, 0, xts, hb, [banks[6], banks[7]], junk, ss, rstd)
    for h in range(4):
        pb = banks[h % 2]
        for kc in range(8):
            b.mm(pb[:, 0:256], wkv[:, kc, h * 128:(h + 1) * 128], memT[:, kc, :], kc == 0, kc == 7)
        b.copy(kmT[:, h, :], pb[:, 0:256])
    for mt in range(2):
        pb = banks[2 + mt]
        for kc in range(8):
            b.mm(pb[:, :], memT[:, kc, mt * 128:(mt + 1) * 128], wkv[:, kc, 512:1024], kc == 0, kc == 7)
        b.copy(vm[:, mt, :], pb[:, :])
    norm_tiles(x_d, 0, NT, prew, hT, 0, xts, hb, [banks[6], banks[7]], junk, ss, rstd)

    S.barrier()
    b.sb_off = a_mark
    qaug = [b.sb(f"qaug{j}", [84, T], BF16) for j in range(2)]
    kaug = [b.sb(f"kaug{j}", [84, T], BF16) for j in range(2)]
    vP = b.sb("vP", [128, 32, 2, 128], BF16)
    zs = b.sb("zs", [128, T], BF16)
    yaT = b.sb("yaT", [128, T], BF16)
    trib = b.sb("trib", [128, 128], BF16)
    pastb = b.sb("pastb", [128, 512], F32)
    Gs = b.sb("Gs", [128, 512], F32)
    mbias2 = [b.sb(f"mbias{j}", [128, 512], F32) for j in range(2)]
    top8 = b.sb("top8", [128, 32, 8], F32)
    ksum = b.sb("ksum", [64, 32], F32)
    kbarT = [b.sb(f"kbarT{j}", [64, 16], BF16) for j in range(2)]
    PT = [b.sb(f"PT{i}", [128, 512], BF16) for i in range(4)]
    rec = b.sb("rec", [64, 512], F32)
    lnd = b.sb("lnd", [64, 512], F32)
    tmpn = b.sb("tmpn", [128, 512], F32)
    thz = [b.sb(f"thz{i}", [128, 512], F32) for i in range(2)]
    b.dma("sp", trib[:], trib_d)
    b.dma("sp", pastb[:], pastb_d)
    b.memset(vP[:, :, :, 64:128].rearrange("p t j c -> p (t j) c"), 1.0)
    for j in range(2):
        b.dma("sp", kaug[j][64:84, :], kac_d)
    pj = [banks[0], banks[1]]
    sbk = [banks[0], banks[1], banks[2], banks[3]]
    ob = [banks[4], banks[5]]
    gb_ = banks[6]
    tb_ = banks[7]

    for p in range(4):
        cs = slice(p * 128, (p + 1) * 128)
        for j in range(2):
            b.dma("sp", qaug[j][80:84, :], qac_d[2 * p + j])
        for tc in range(8):
            ts_ = slice(tc * 512, (tc + 1) * 512)
            pb = banks[0]
            for kc in range(8):
                b.mm(pb[:, :], wq[:, kc, cs], hT[:, kc, ts_], kc == 0, kc == 7)
            b.act(qaug[0][0:64, ts_], pb[0:64, :], AF.Copy, scale=0.125)
            b.ts(qaug[1][0:64, ts_], pb[64:128, :], 0.125, None, ALU.mult)
            pb = banks[1]
            for kc in range(8):
                b.mm(pb[:, :], wk[:, kc, cs], hT[:, kc, ts_], kc == 0, kc == 7)
            b.act(kaug[0][0:64, ts_], pb[0:64, :], AF.Copy)
            b.copy(kaug[1][0:64, ts_], pb[64:128, :])
            for j in range(2):
                b.rsum(ksum[0:64, 16 * j + 2 * tc:16 * j + 2 * tc + 2],
                       pb[64 * j:64 * j + 64, :].rearrange("p (n s) -> p n s", s=256))
            pb = banks[2 + tc % 2]
            for kc in range(8):
                b.mm(pb[:, :], wz[:, kc, cs], hT[:, kc, ts_], kc == 0, kc == 7)
            b.act(thz[tc % 2][:, :], pb[:, :], AF.Tanh, scale=0.5)
            b.stt(zs[:, ts_], thz[tc % 2][:, :], 1.0, pb[:, :], ALU.add, ALU.mult)
        b.ts(kbarT[0][:, :], ksum[:, 0:16], 1.0 / 256, None, ALU.mult)
        b.ts(kbarT[1][:, :], ksum[:, 16:32], 1.0 / 256, None, ALU.mult)
        for j in range(2):
            for tt_ in range(NT):
                b.mm(gb_[:, tt_ * 16:(tt_ + 1) * 16], qaug[j][0:64, tt_ * 128:(tt_ + 1) * 128], kbarT[j][:, :])
            b.tt(Gs[:, :], gb_[:, :], pastb[:, :], ALU.add)
            for tt_ in range(NT):
                b.max8(top8[:, tt_, :], Gs[:, tt_ * 16:(tt_ + 1) * 16])
            mb = mbias2[j]
            b.tt(mb[:, :].rearrange("p (t n) -> p t n", n=16), Gs[:, :].rearrange("p (t n) -> p t n", n=16),
                 bc_last(top8[:, :, 2], 16), ALU.is_lt)
            b.ts(mb[:, :], mb[:, :], NEG, None, ALU.mult)
            own = bass.AP(mb, 0, [[512, 128], [33, 16], [16, 2]])
            b.memset(own, 0.0)
        for g in range(8):
            pb = banks[4 + g % 2]
            for i in range(4):
                tt_ = g * 4 + i
                for kc in range(8):
                    b.mm(pb[:, i * 128:(i + 1) * 128], hT[:, kc, tt_ * 128:(tt_ + 1) * 128], wv[:, kc, cs],
                         kc == 0, kc == 7)
            b.act(vP[:, g * 4:(g + 1) * 4, :, 0:64], pb[:, :].rearrange("p (t j c) -> p t j c", j=2, c=64), AF.Copy)
        if p == 3:
            for w, c0 in ((wq, 2048), (wz, 3584), (wk, 3072), (wv, 2560)):
                load_w(w, win_d, c0, 512, 8)
        for j in range(2):
            mb = mbias2[j]
            for g in range(8):
                for i in range(4):
                    tt_ = g * 4 + i
                    b.tr(tb_[0:16, i * 128:(i + 1) * 128], mb[:, tt_ * 16:(tt_ + 1) * 16], identf[:])
                b.copy(qaug[j][64:80, g * 512:(g + 1) * 512], tb_[0:16, :])
        for j in range(2):
            q_, k_ = qaug[j], kaug[j]
            iters = [(tc, st) for tc in range(8) for st in range(4 * tc + 4)]

            def qk(n):
                tc, st = iters[n]
                dj = st - 4 * tc
                c0 = 128 * dj if dj >= 0 else 0
                Sb = sbk[n % 4]
                kl = k_[0:84, st * 128:(st + 1) * 128]
                if dj < 0:
                    b.mm(Sb[:, :], kl, q_[0:84, tc * 512:(tc + 1) * 512])
                else:
                    b.mm(Sb[:, c0:c0 + 128], identb[:], trib[:], True, False)
                    b.mm(Sb[:, c0:c0 + 128], kl, q_[0:84, tc * 512 + c0:tc * 512 + c0 + 128], False, True)
                    if c0 + 128 < 512:
                        b.mm(Sb[:, c0 + 128:512], kl, q_[0:84, tc * 512 + c0 + 128:(tc + 1) * 512])

            qk(0)
            qk(1)
            qk(2)
            for n, (tc, st) in enumerate(iters):
                if n + 3 < len(iters):
                    qk(n + 3)
                dj = st - 4 * tc
                c0 = 128 * dj if dj >= 0 else 0
                nst = 4 * tc + 4
                O = ob[tc % 2]
                b.act(PT[n % 4][:, c0:512], sbk[n % 4][:, c0:512], AF.Exp)
                b.mm(O[:, c0:512], vP[:, st, j, :], PT[n % 4][:, c0:512], st == 0, st == nst - 1)
                if st == nst - 1:
                    ts_ = slice(tc * 512, (tc + 1) * 512)
                    rr = slice(64 * j, 64 * j + 64)
                    b.recip(rec[0:64, :], O[64:128, :])
                    b.tt(tmpn[rr, :], O[0:64, :], rec[0:64, :], ALU.mult)
                    b.stt(yaT[rr, ts_], tmpn[rr, :], 0.5, zs[rr, ts_], ALU.mult, ALU.mult)
        b.dma("sp", ya_d[p * 128:(p + 1) * 128, :], yaT[:, :])

    S.barrier()
    b.sb_off = a_mark
    wf, wg, wqh, wi = wq, wz, wk, wv
    TOP = 188352
    wqc = b.sb("wqc", [128, 8, 512], BF16, off=TOP)
    wzc = b.sb("wzc", [128, 8, 512], BF16, off=TOP + 8192)
    wa = b.sb("wa", [128, 4, 1024], BF16, off=TOP + 16384)
    wb = b.sb("wb", [128, 4, 1024], BF16, off=TOP + 24576)
    wc = b.sb("wc", [128, 4, 1024], BF16, off=TOP + 32768)
    load_w(wqc, win_d, 4096, 512, 8)
    load_w(wzc, win_d, 4608, 512, 8)
    load_w(wa, wa_d, 0, 1024, 4)
    load_w(wb, wb_d, 0, 1024, 4)
    load_w(wc, wc_d, 0, 1024, 4)
    lbl = b.sb("lbl", [128, 8], F32)
    lb = b.sb("lb", [128, 4], F32)
    homl = b.sb("homl", [128, 4], F32)
    nhoml = b.sb("nhoml", [128, 4], F32)
    lnb = b.sb("lnb", [128, 4], F32)
    hnw = b.sb("hnw", [128, 1], F32)
    hnwh = b.sb("hnwh", [128, 1], F32)
    trim = b.sb("trim", [128, 128], F32)
    rmask = b.sb("rmask", [128, 512], F32)
    b.dma("sp", lbl[:], lbl_d)
    b.dma("sp", hnw[:], hnw_d)
    b.dma("sp", trim[:], trim_d)
    b.dma("sp", rmask[:], rmask_d)
    b.tt(lb[:, :], lbl[:, 0:4], lbl[:, 4:8], ALU.subtract)
    b.act(lb[:, :], lb[:, :], AF.Tanh, scale=0.5)
    b.ts(lb[:, :], lb[:, :], 0.5, 0.5, ALU.mult, ALU.add)
    b.ts(homl[:, :], lb[:, :], -0.5, 0.5, ALU.mult, ALU.add)
    b.ts(nhoml[:, :], homl[:, :], -1.0, None, ALU.mult)
    b.tt(lnb[:, :], homl[:, :], lb[:, :], ALU.add)
    b.ts(hnwh[:, :], hnw[:, :], 0.5, None, ALU.mult)

    def two(name, shape, dt):
        return [b.sb(f"{name}{i}", shape, dt) for i in range(2)]
    thf = [b.sb("thf", [128, 512], F32)] * 2
    thg = [b.sb("thg", [128, 512], F32)] * 2
    gs2 = two("gs2", [128, 512], F32)
    kk = two("kk", [128, 512], F32)
    lf = [b.sb("lf", [128, 512], F32)] * 2
    bb = two("bb", [128, 512], F32)
    eb = two("eb", [128, 512], F32)
    enb = two("enb", [128, 512], F32)
    edl = two("edl", [128, 512], F32)
    qt = two("qt", [128, 512], BF16)
    qsb = two("qsb", [128, 512], F32)
    kt = two("kt", [128, 512], BF16)
    kdT = two("kdT", [128, 512], BF16)
    vtm = two("vtm", [128, 512], BF16)
    kd = b.sb("kd", [128, 512], BF16)
    ATm = b.sb("ATm", [128, 512], BF16)
    Sst = b.sb("Sst", [128, 128], F32)
    SbfA = two("SbfA", [128, 4, 128], BF16)
    sq = b.sb("sq", [128, 512], BF16)
    rl = b.sb("rl", [128, 512], F32)
    rs2 = b.sb("rs2", [128, 512], F32)
    obn = b.sb("obn", [128, 512], F32)
    ybT = b.sb("ybT", [128, T], BF16)
    assert b.sb_off <= TOP, b.sb_off
    pf_, pq_, pg_, pv_, pAT, pU, po_, px = banks
    its = [(hh, tc) for hh in range(4) for tc in range(8)]

    def stage1_pe(it, part):
        hh, tc = its[it]
        cs = slice(hh * 128, (hh + 1) * 128)
        ts_ = slice(tc * 512, (tc + 1) * 512)
        if part == 0:
            for kc in range(8):
                b.mm(pf_[:, :], wf[:, kc, cs], hT[:, kc, ts_], kc == 0, kc == 7)
        elif part == 1:
            for kc in range(8):
                b.mm(pg_[:, :], wg[:, kc, cs], hT[:, kc, ts_], kc == 0, kc == 7)
        elif part == 2:
            for kc in range(8):
                b.mm(pq_[:, :], wqh[:, kc, cs], hT[:, kc, ts_], kc == 0, kc == 7)
        else:
            for i in range(4):
                tt_ = tc * 4 + i
                for kc in range(8):
                    b.mm(pv_[:, i * 128:(i + 1) * 128], hT[:, kc, tt_ * 128:(tt_ + 1) * 128], wi[:, kc, cs],
                         kc == 0, kc == 7)

    def stage1_early(it, part=None):
        hh, tc = its[it]
        s_ = it % 2
        if part in (None, 0):
            b.act(thf[s_][:, :], pf_[:, :], AF.Tanh, scale=0.5)
            b.act(thg[s_][:, :], pg_[:, :], AF.Tanh, scale=0.5)
        if part in (None, 1):
            b.act(vtm[s_][:, :], pv_[:, :], AF.Copy)
            b.stt(gs2[s_][:, :], thg[s_][:, :], 1.0, pg_[:, :], ALU.add, ALU.mult)
        if part in (None, 2):
            b.copy(qsb[s_][:, :], pq_[:, :])

    def stage1_ew(it):
        hh, tc = its[it]
        s_ = it % 2
        b.ts(kk[s_][:, :], thf[s_][:, :], nhoml[:, hh:hh + 1], homl[:, hh:hh + 1], ALU.mult, ALU.add)
        b.act(lf[s_][:, :], thf[s_][:, :], AF.Ln, bias=lnb[:, hh:hh + 1], scale=homl[:, hh:hh + 1])
        b.scan(bb[s_][:, :], rmask[:, :], lf[s_][:, :], 0.0, ALU.mult, ALU.add)
        b.act(eb[s_][:, :], bb[s_][:, :], AF.Exp)
        b.act(enb[s_][:, :], bb[s_][:, :], AF.Exp, scale=-1.0)
        for c in range(4):
            b.act(edl[s_][:, c * 128:(c + 1) * 128], bb[s_][:, c * 128:(c + 1) * 128], AF.Exp,
                  bias=bb[s_][:, c * 128 + 127:c * 128 + 128], scale=-1.0)
        b.tt(qt[s_][:, :], qsb[s_][:, :], eb[s_][:, :], ALU.mult)
        b.tt(kt[s_][:, :], kk[s_][:, :], enb[s_][:, :], ALU.mult)
        b.tt(kdT[s_][:, :], kk[s_][:, :], edl[s_][:, :], ALU.mult)

    def stage2(it, part):
        hh, tc = its[it]
        s_ = it % 2
        pxb = px[:].bitcast(BF16)
        if part == 0:
            if tc == 0:
                b.memset(Sst[:], 0.0)
                b.memset(SbfA[s_][:, 0, :], 0.0)
            for c in range(4):
                b.tr(pxb[:, c * 128:(c + 1) * 128], kdT[s_][:, c * 128:(c + 1) * 128], identb[:])
            b.copy(kd[:, :], pxb[:, 0:512])
            for c in range(4):
                cc = slice(c * 128, (c + 1) * 128)
                b.mm(pAT[:, cc], kt[s_][:, cc], qt[s_][:, cc])
            trim4 = bass.AP(trim, 0, [[128, 128], [0, 4], [1, 128]])
            b.tt(ATm[:, :].rearrange("p (c t) -> p c t", t=128), pAT[:, :].rearrange("p (c t) -> p c t", t=128),
                 trim4, ALU.mult)
        elif part == 1:
            for c in range(4):
                cc = slice(c * 128, (c + 1) * 128)
                b.mm(pU[:, cc], kd[:, cc], vtm[s_][:, cc])
            for c in range(4):
                cc = slice(c * 128, (c + 1) * 128)
                b.stt(Sst[:, :], Sst[:, :], eb[s_][:, c * 128 + 127:c * 128 + 128], pU[:, cc], ALU.mult, ALU.add)
                dst = SbfA[s_][:, c + 1, :] if c < 3 else SbfA[1 - s_][:, 0, :]
                b.copy(dst, Sst[:, :])
        else:
            for c in range(4):
                cc = slice(c * 128, (c + 1) * 128)
                b.mm(po_[:, cc], vtm[s_][:, cc], ATm[:, cc], True, False)
                b.mm(po_[:, cc], SbfA[s_][:, c, :], qt[s_][:, cc], False, True)
            b.act(sq[:, :], po_[:, :], AF.Square)

    def stage2b(it):
        hh, tc = its[it]
        s_ = it % 2
        ts_ = slice(tc * 512, (tc + 1) * 512)
        b.mm(px[:, :], onesb[:, :], sq[:, :])
        b.act(rl[:, :], px[:, :], AF.Ln, bias=epsb[:, 0:1], scale=1.0 / 128)
        b.act(rs2[:, :], rl[:, :], AF.Exp, scale=-0.5)
        b.tt(obn[:, :], po_[:, :], rs2[:, :], ALU.mult)
        b.stt(ybT[:, ts_], obn[:, :], hnwh[:, 0:1], gs2[s_][:, :], ALU.mult, ALU.mult)
        if tc == 7:
            b.dma("sp", yb_d[hh * 128:(hh + 1) * 128, :], ybT[:, :])

    for part in range(4):
        stage1_pe(0, part)
    stage1_early(0)
    stage1_ew(0)
    for it in range(len(its)):
        nxt = it + 1 < len(its)
        if HGRN_FINE:
            if nxt:
                stage1_pe(it + 1, 0)
            stage2(it, 0)
            if nxt:
                stage1_pe(it + 1, 1)
            if it > 0:
                stage2b(it - 1)
            stage2(it, 1)
            if nxt:
                stage1_pe(it + 1, 2)
            stage2(it, 2)
            if nxt:
                stage1_pe(it + 1, 3)
                stage1_early(it + 1)
                stage1_ew(it + 1)
        else:
            if nxt:
                for part in range(2):
                    stage1_pe(it + 1, part)
            if it > 0:
                stage2b(it - 1)
            if nxt:
                stage1_early(it + 1, 0)
            stage2(it, 0)
            if nxt:
                stage1_pe(it + 1, 3)
                stage1_early(it + 1, 1)
            stage2(it, 1)
            if nxt:
                stage1_pe(it + 1, 2)
                stage1_early(it + 1, 2)
            stage2(it, 2)
            if nxt:
                stage1_ew(it + 1)
    stage2b(len(its) - 1)

    S.barrier()
    b.sb_off = persist_mark
    wga = b.sb("wga", [128, 8, 1024], BF16)
    wgb = b.sb("wgb", [128, 8, 1024], BF16)
    wgc = b.sb("wgc", [128, 8, 1024], BF16)
    wo = b.sb("wo", [128, 8, 1024], BF16)
    for i, w in enumerate((wga, wgb, wgc)):
        load_w(w, win_d, 5120 + i * 1024, 1024, 8)
    load_w(wo, wo_d, 0, 1024, 8)
    postw = b.sb("postw", [128, D], F32)
    b.dma("sp", postw[:], postw_d)
    eps4 = b.sb("eps4", [128, 1], F32)
    b.memset(eps4[:], 4.0 * EPS)
    hTc = b.sb("hTc", [128, 8, 512], BF16)
    xn = [b.sb(f"xn{i}", [128, D], F32) for i in range(3)]
    xr = [b.sb(f"xr{i}", [128, D], F32) for i in range(2)]
    hbN = [b.sb(f"hbN{i}", [128, D], BF16) for i in range(4)]
    junk = b.sb("junk2", [128, 512], BF16)
    ssn = b.sb("ssn", [128, 4], F32)
    rstdn = b.sb("rstdn", [128, 4], F32)
    mhalf = b.sb("mhalf", [128, 1], F32)
    b.memset(mhalf[:], -0.5)
    ss2 = two("ss3", [128, 2], F32)
    rstd2 = two("rstd3", [128, 1], F32)
    yac = b.sb("yac", [128, 4, 512], BF16)
    ybc = b.sb("ybc", [128, 4, 512], BF16)
    ycc = b.sb("ycc", [128, 4, 512], BF16)
    qcT = [b.sb(f"qcT{i}", [128, 512], BF16) for i in range(4)]
    thc = two("thc", [128, 512], F32)
    zs2 = [b.sb(f"zs2c{i}", [128, 512], F32) for i in range(4)]
    PX = [b.sb(f"PX{i}", [128, 512], BF16) for i in range(4)]
    recx = two("recx", [128, 512], F32)
    tmpx2 = [m1, m2] = two("tmpx", [128, 512], F32)
    sgs = [b.sb(f"sgs{i}", [128, 512], F32) for i in range(4)]
    mT = b.sb("mT", [128, 8, 512], BF16)
    tmpo = recx[0]
    assert b.sb_off <= TOP, b.sb_off
    yav = ya_d.rearrange("(k p) t -> p k t", p=128)
    ybv = yb_d.rearrange("(k p) t -> p k t", p=128)
    gcount = 0
    xn_i = [0]
    xn_of = {}

    def nA_load(tc, i):
        k = xn_i[0] % 3
        xn_i[0] += 1
        xn_of[(tc, i)] = xn[k]
        r1 = tc * 512 + i * 128
        b.dma("sp", xn[k][:], x_d[r1:r1 + 128, :])

    def nA_stats(tc, i):
        xt = xn_of[(tc, i)]
        b.act(hbN[i][:], xt[:], AF.Square, accum_out=ssn[:, i:i + 1])
        b.ts(rstdn[:, i:i + 1], ssn[:, i:i + 1], 1.0 / D, EPS, ALU.mult, ALU.add, eng="pool")
        b.tt(rstdn[:, i:i + 1], rstdn[:, i:i + 1], mhalf[:, 0:1], ALU.pow, eng="pool")

    def nA_scale(tc, i):
        xt = xn_of[(tc, i)]
        b.act(hbN[i][:], xt[:], AF.Copy, scale=rstdn[:, i:i + 1])

    def normB(i):
        tpv = banks[6 + i % 2][:].bitcast(BF16)
        for kc in range(8):
            b.tr(tpv[:, kc * 128:(kc + 1) * 128], hbN[i][:, kc * 128:(kc + 1) * 128], identb[:])
        b.tt(hTc[:, :, i * 128:(i + 1) * 128], tpv.rearrange("p (k t) -> p k t", t=128),
             bc_last(prew[:, :], 128), ALU.mult)

    def normA_steps(tc):
        return [
            lambda: (nA_load(tc, 0), nA_load(tc, 1), nA_load(tc, 2), nA_stats(tc, 0)),
            lambda: (nA_stats(tc, 1), nA_scale(tc, 0), nA_load(tc, 3)),
            lambda: (nA_stats(tc, 2), nA_scale(tc, 1)),
            lambda: (nA_stats(tc, 3), nA_scale(tc, 2)),
            lambda: (nA_scale(tc, 3),),
        ]

    def xr_load(tc, i):
        r1 = tc * 512 + i * 128
        b.dma("sp", xr[i % 2][:], x_d[r1:r1 + 128, :])

    for st_ in normA_steps(0):
        st_()
    b.dma("sp", yac[:, :, :], yav[:, :, 0:512])
    b.dma("sp", ybc[:, :, :], ybv[:, :, 0:512])
    for i in range(4):
        normB(i)
    for tc in range(8):
        ts_ = slice(tc * 512, (tc + 1) * 512)
        xr_load(tc, 0)
        xr_load(tc, 1)
        for h in range(4):
            cs = slice(h * 128, (h + 1) * 128)
            for kc in range(8):
                b.mm(banks[h][:, :], wqc[:, kc, cs], hTc[:, kc, :], kc == 0, kc == 7)
            b.act(qcT[h][:, :], banks[h][:, :], AF.Copy, scale=float(128 ** -0.5))

        def zproj(h, bank):
            cs = slice(h * 128, (h + 1) * 128)
            for kc in range(8):
                b.mm(bank[:, :], wzc[:, kc, cs], hTc[:, kc, :], kc == 0, kc == 7)
            b.act(thc[h % 2][:, :], bank[:, :], AF.Tanh, scale=0.5)
            b.stt(zs2[h][:, :], thc[h % 2][:, :], 1.0, bank[:, :], ALU.add, ALU.mult)

        def lmm(h0):
            for hh_ in (h0, h0 + 1):
                for mt in range(2):
                    k = 2 * (hh_ - h0) + mt
                    b.mm(banks[k][:, :], kmT[:, hh_, mt * 128:(mt + 1) * 128], qcT[hh_][:, :])
                    b.act(PX[k][:, :], banks[k][:, :], AF.Exp)

        def pvn(h0):
            for hh_ in (h0, h0 + 1):
                cs = slice(hh_ * 128, (hh_ + 1) * 128)
                O = banks[4 + 2 * (hh_ - h0)]
                Dn = banks[5 + 2 * (hh_ - h0)]
                for mt in range(2):
                    b.mm(O[:, :], vm[:, mt, cs], PX[2 * (hh_ - h0) + mt][:, :], mt == 0, mt == 1)
                for mt in range(2):
                    b.mm(Dn[:, :], onesb[:, :], PX[2 * (hh_ - h0) + mt][:, :], mt == 0, mt == 1)
            for hh_ in (h0, h0 + 1):
                O = banks[4 + 2 * (hh_ - h0)]
                Dn = banks[5 + 2 * (hh_ - h0)]
                tx = tmpx2[hh_ - h0]
                b.act(tx[:, :], Dn[:, :], AF.Ln)
                b.act(recx[hh_ - h0][:, :], tx[:, :], AF.Exp, scale=-1.0)
                b.tt(tx[:, :], O[:, :], recx[hh_ - h0][:, :], ALU.mult)
                b.stt(ycc[:, hh_, :], tx[:, :], 0.5, zs2[hh_][:, :], ALU.mult, ALU.mult)

        zproj(0, banks[4])
        zproj(1, banks[5])
        lmm(0)
        zproj(2, banks[6])
        zproj(3, banks[7])
        pvn(0)
        lmm(2)
        pvn(2)
        nsteps = normA_steps(tc + 1) if tc + 1 < 8 else []
        for oc in range(8):
            os_ = slice(oc * 128, (oc + 1) * 128)
            Pbs = []
            sg_ = []
            for bi, (wgx, wx, yx) in enumerate(((wga, wa, yac), (wgb, wb, ybc), (wgc, wc, ycc))):
                G = banks[gcount % 2]
                Pb = banks[2 + gcount % 4]
                sgb = sgs[gcount % 4]
                gcount += 1
                for kc in range(8):
                    b.mm(G[:, :], wgx[:, kc, os_], hTc[:, kc, :], kc == 0, kc == 7)
                b.act(sgb[:, :], G[:, :], AF.Tanh, scale=0.5)
                for k4 in range(4):
                    b.mm(Pb[:, :], wx[:, k4, os_], yx[:, k4, :], k4 == 0, k4 == 3)
                Pbs.append(Pb)
                sg_.append(sgb)
            b.stt(m1[:, :], sg_[0][:, :], 1.0, Pbs[0][:, :], ALU.add, ALU.mult)
            b.stt(m2[:, :], sg_[1][:, :], 1.0, Pbs[1][:, :], ALU.add, ALU.mult)
            b.tt(m1[:, :], m1[:, :], m2[:, :], ALU.add)
            b.stt(m2[:, :], sg_[2][:, :], 1.0, Pbs[2][:, :], ALU.add, ALU.mult)
            b.tt(mT[:, oc, :], m1[:, :], m2[:, :], ALU.add)
            if 1 <= oc <= 5 and nsteps:
                nsteps[oc - 1]()
        if tc + 1 < 8:
            tn_ = slice((tc + 1) * 512, (tc + 2) * 512)
            b.dma("sp", yac[:, :, :], yav[:, :, tn_])
            b.dma("sp", ybc[:, :, :], ybv[:, :, tn_])
        for i in range(4):
            xt = xr[i % 2]
            r0 = tc * 512 + i * 128
            Y = [banks[(2 * i) % 4], banks[(2 * i) % 4 + 1]]
            s2 = ss2[i % 2]
            r2 = rstd2[i % 2]
            for hf in range(2):
                for kc in range(8):
                    b.mm(Y[hf][:, :], mT[:, kc, i * 128:(i + 1) * 128], wo[:, kc, hf * 512:(hf + 1) * 512],
                         kc == 0, kc == 7)
                b.act(junk[:, :], Y[hf][:, :], AF.Square, accum_out=s2[:, hf:hf + 1])
            if tc + 1 < 8:
                normB(i)
            b.tt(r2[:, 0:1], s2[:, 0:1], s2[:, 1:2], ALU.add, eng="pool")
            b.ts(r2[:, 0:1], r2[:, 0:1], 1.0 / D, 4.0 * EPS, ALU.mult, ALU.add, eng="pool")
            b.tt(r2[:, 0:1], r2[:, 0:1], mhalf[:, 0:1], ALU.pow, eng="pool")
            for hf in range(2):
                b.stt(tmpo[:, :], Y[hf][:, :], r2[:, 0:1], postw[:, hf * 512:(hf + 1) * 512], ALU.mult, ALU.mult)
                b.tt(xt[:, hf * 512:(hf + 1) * 512], xt[:, hf * 512:(hf + 1) * 512], tmpo[:, :], ALU.add)
            b.dma("sp", out_d[r0:r0 + 128, :], xt[:])
            if i + 2 < 4:
                xr_load(tc, i + 2)
    S.final_wait()

    from contextlib import ExitStack
    with ExitStack() as es:
        sems = {e: es.enter_context(nc.semaphore(f"s_{e}")) for e in Sched.CE}
        dma_sems = {
            "sp": [es.enter_context(nc.semaphore(f"d_sp{i}")) for i in range(24)],
            "pool": [es.enter_context(nc.semaphore(f"d_pl{i}")) for i in range(16)],
        }
        block = es.enter_context(nc.Block())
        S.emit(block, sems, dma_sems)
    return nc


def _consts():
    bf = ml_dtypes.bfloat16
    c = {}
    c["identb"] = np.eye(128, dtype=np.float32).astype(bf)
    c["identf"] = np.eye(128, dtype=np.float32)
    s = np.arange(128)[:, None]
    t = np.arange(128)[None, :]
    c["trib"] = np.where(s <= t, 0.0, NEG).astype(np.float32).astype(bf)
    c["trim"] = (s <= t).astype(np.float32)
    pos = np.arange(T)
    kac = np.zeros((20, T), np.float32)
    for n in range(16):
        kac[n] = (pos // 256 == n)
    kac[16] = pos % 128
    kac[17] = (pos // 128) * 128
    kac[18] = 1.0
    kac[19] = 1.0
    c["kac"] = kac.astype(bf)
    qac = np.zeros((8, 4, T), np.float32)
    for h in range(8):
        slope = 2.0 ** (-(h + 1))
        qac[h, 0] = slope
        qac[h, 1] = slope
        qac[h, 2] = -slope * (pos % 128)
        qac[h, 3] = -slope * ((pos // 128) * 128)
    c["qac"] = qac.astype(bf)
    pastb = np.zeros((128, 32, 16), np.float32)
    for tt in range(32):
        pastb[:, tt, tt // 2:] = -1e30
    c["pastb"] = pastb.reshape(128, 512)
    rm = np.ones((128, 512), np.float32)
    rm[:, ::128] = 0.0
    c["rmask"] = rm
    return c


_NC = [None]


def kernel(x, mem, pre_norm_w, w_in, hgrn_lb_logits, hgrn_norm_w, mem_norm_w, w_mem_kv,
           w_branch_a, w_branch_b, w_branch_c, w_out, post_norm_w):
    f = lambda a: np.ascontiguousarray(np.asarray(a, dtype=np.float32))
    x = f(x)
    mem = f(mem)
    if _NC[0] is None:
        _NC[0] = build()
    nc = _NC[0]
    shared = dict(_consts())
    shared["w_in"] = f(w_in)[0]
    shared["w_kv"] = f(w_mem_kv)[0]
    shared["w_a"] = f(w_branch_a)[0]
    shared["w_b"] = f(w_branch_b)[0]
    shared["w_c"] = f(w_branch_c)[0]
    shared["w_o"] = f(w_out)[0]
    shared["prew"] = np.ascontiguousarray(f(pre_norm_w)[0].reshape(8, 128).T)
    shared["memw"] = np.ascontiguousarray(f(mem_norm_w)[0].reshape(8, 128).T)
    shared["postw"] = np.ascontiguousarray(np.broadcast_to(f(post_norm_w)[0][None, :], (128, D)))
    lbl = f(hgrn_lb_logits).reshape(2, 4, 128)
    shared["lbl"] = np.ascontiguousarray(lbl.transpose(2, 0, 1).reshape(128, 8))
    shared["hnw"] = np.ascontiguousarray(f(hgrn_norm_w)[0].reshape(128, 1))
    in_maps = []
    for c in range(8):
        m = dict(shared)
        m["x"] = x[c]
        m["mem"] = mem[c]
        in_maps.append(m)
    res = run_bass_kernel_spmd(nc, in_maps, core_ids=list(range(8)))
    kernel.last = res
    return np.stack([r["out"] for r in res.results], axis=0).astype(np.float32)
```
